# Optimizing a Trainium2 kernel written in Bass

```python
import math
import jax
import jax.numpy as jnp
from jax import lax
import numpy as np

D_MODEL = 2048
BATCH = 4
SEQ = 2048
DEPTH = 2

GRID_W = 64
CTX_LEN = 256
EPS = 1e-6
N_MIXERS = 4
MIX_WIDTH = D_MODEL
GROUP_W = MIX_WIDTH // N_MIXERS
DN_HEADS = 4
DN_HEAD_DIM = GROUP_W // DN_HEADS
DN_CHUNK = 64
SHORT_CONV = 5
SG_CHUNK = 128
SG_HEADS = 4
SG_HEAD_DIM = GROUP_W // SG_HEADS
POOL_WINDOWS = (2, 4, 8, 16)
POOL_GROUP_DIM = GROUP_W // len(POOL_WINDOWS)
CONF_CONV = 31
QKV_COLS = 3 * GROUP_W
AB_COLS = 4 * DN_HEADS
DN_STATE_COLS = QKV_COLS + AB_COLS
Z_COLS = GROUP_W
SG_COLS = 2 * GROUP_W
POOL_COLS = GROUP_W
CV_COLS = 2 * GROUP_W
IN_WIDTH = DN_STATE_COLS + Z_COLS + SG_COLS + POOL_COLS + CV_COLS
D_FF = 256 * ((8 * D_MODEL // 3 + 255) // 256)
N_EXPERTS = 8
TOP_K = 2
D_FF_EXPERT = 7 * D_MODEL // 2
N_DENSE = (DEPTH + 1) // 2
N_MOE = DEPTH // 2

kernel_name = 'hybrid_headgroup_diffusion_trunk'

F32 = jnp.float32


def rmsnorm(x, g):
    xf = x.astype(F32)
    y = xf * lax.rsqrt(jnp.mean(xf * xf, axis=-1, keepdims=True) + EPS)
    return (y * g.astype(F32)).astype(x.dtype)


def layernorm(x, g, b):
    xf = x.astype(F32)
    mu = jnp.mean(xf, axis=-1, keepdims=True)
    xc = xf - mu
    var = jnp.mean(xc * xc, axis=-1, keepdims=True)
    return (xc * lax.rsqrt(var + EPS) * g.astype(F32) + b.astype(F32)).astype(x.dtype)


def l2norm(x):
    return x * lax.rsqrt(jnp.sum(x * x, axis=-1, keepdims=True) + EPS)


def modulate(h, shift, scale):
    return h * (1 + scale) + shift


def depthwise_conv(x, w):
    pad = w.shape[0] // 2
    return lax.conv_general_dilated(
        x, w.astype(x.dtype)[:, None, :], window_strides=(1,), padding=[(pad, pad)],
        dimension_numbers=('NWC', 'WIO', 'NWC'), feature_group_count=x.shape[-1])


def grid_pos_embed(rows, dim):
    r = jnp.repeat(jnp.arange(rows, dtype=F32), GRID_W)
    col = jnp.tile(jnp.arange(GRID_W, dtype=F32), rows)
    quarter = dim // 4
    freq = jnp.exp(-math.log(10000.0) * jnp.arange(quarter, dtype=F32) / quarter)
    ar = r[:, None] * freq
    ac = col[:, None] * freq
    return jnp.concatenate([jnp.sin(ar), jnp.cos(ar), jnp.sin(ac), jnp.cos(ac)], axis=-1)


def delta_chunked(q, k, v, log_a, beta, s0):
    B_, T, H, _ = q.shape
    C = DN_CHUNK
    N = T // C

    def chunk(t):
        t = t.reshape((B_, N, C, H) + t.shape[3:])
        return jnp.moveaxis(t, (1, 3), (0, 2))

    qc, kc, vc = chunk(q), chunk(k), chunk(v)
    g = jnp.cumsum(chunk(log_a), axis=-1)
    bc = chunk(beta)[..., None]
    kb = kc * bc
    vb = vc * bc
    idx = jnp.arange(C)
    lower = idx[:, None] >= idx[None, :]
    strict = idx[:, None] > idx[None, :]
    gdiff = g[..., :, None] - g[..., None, :]
    decay = jnp.where(lower, jnp.exp(jnp.where(lower, gdiff, 0.0)), 0.0)
    lmat = jnp.where(strict, jnp.einsum('nbhid,nbhjd->nbhij', kb, kc) * decay, 0.0)
    amat = jnp.eye(C, dtype=F32) + lmat
    rhs = jnp.concatenate([vb, kb * jnp.exp(g)[..., None]], axis=-1)
    sol = lax.linalg.triangular_solve(amat, rhs, left_side=True, lower=True)
    dv = v.shape[-1]
    u = sol[..., :dv]
    w = sol[..., dv:]
    qk = jnp.where(lower, jnp.einsum('nbhid,nbhjd->nbhij', qc, kc) * decay, 0.0)
    g_last = g[..., -1]

    def step(S, inp):
        qi, ki, ui, wi, gi, gl, qki = inp
        v_new = ui - jnp.einsum('bhcd,bhde->bhce', wi, S)
        o = (jnp.einsum('bhcd,bhde->bhce', qi * jnp.exp(gi)[..., None], S)
             + jnp.einsum('bhij,bhje->bhie', qki, v_new))
        S = (S * jnp.exp(gl)[..., None, None]
             + jnp.einsum('bhcd,bhce->bhde', ki * jnp.exp(gl[..., None] - gi)[..., None], v_new))
        return S, o

    S, o = lax.scan(step, s0, (qc, kc, u, w, g, g_last, qk))
    o = jnp.moveaxis(o, (0, 2), (1, 3)).reshape(B_, T, H, dv)
    return o, S


def dn_prepare(p, conv_w, a_log, dt_bias):
    B_, T, _ = p.shape
    qkv = jax.nn.silu(depthwise_conv(p[..., :QKV_COLS], conv_w)).astype(F32)
    qkv = qkv.reshape(B_, T, 3, DN_HEADS, DN_HEAD_DIM)
    q = l2norm(qkv[:, :, 0]) * (DN_HEAD_DIM ** -0.5)
    k = l2norm(qkv[:, :, 1])
    v = qkv[:, :, 2]
    ab = p[..., QKV_COLS:DN_STATE_COLS].astype(F32).reshape(B_, T, 4, DN_HEADS)
    log_a = -jnp.exp(a_log.astype(F32)) * jax.nn.softplus(ab[:, :, 0:2] + dt_bias.astype(F32))
    beta = jax.nn.sigmoid(ab[:, :, 2:4])
    return q, k, v, log_a, beta


def dn_bidir_scan(q, k, v, log_a, beta, s0_f, s0_b):
    o_f, s_f = delta_chunked(q, k, v, log_a[:, :, 0], beta[:, :, 0], s0_f)
    fl = lambda t: jnp.flip(t, axis=1)
    o_b, s_b = delta_chunked(fl(q), fl(k), fl(v), fl(log_a[:, :, 1]), fl(beta[:, :, 1]), s0_b)
    return o_f + fl(o_b), s_f, s_b


def gated_head_norm(o, z, g):
    B_, T = z.shape[:2]
    zf = z.astype(F32).reshape(B_, T, DN_HEADS, DN_HEAD_DIM)
    y = o * lax.rsqrt(jnp.mean(o * o, axis=-1, keepdims=True) + EPS) * g.astype(F32) * jax.nn.silu(zf)
    return y.reshape(B_, T, GROUP_W).astype(z.dtype)


def spatial_gating(p, ln_g, ln_b, w_s, b_s):
    B_, T, _ = p.shape
    n = T // SG_CHUNK
    u, v = jnp.split(jax.nn.gelu(p), 2, axis=-1)
    v = layernorm(v, ln_g, ln_b).reshape(B_, n, SG_CHUNK, SG_HEADS, SG_HEAD_DIM)
    s = jnp.einsum('hij,bnjhd->bnihd', w_s, v) + b_s.T[None, None, :, :, None]
    return u * s.reshape(B_, T, GROUP_W)


def multiscale_pool(p, pool_w, pool_scale):
    B_, T, _ = p.shape
    xg = p.reshape(B_, T, len(POOL_WINDOWS), POOL_GROUP_DIM)
    cs = jnp.pad(jnp.cumsum(xg.astype(F32), axis=1), ((0, 0), (1, 0), (0, 0), (0, 0)))
    t = jnp.arange(T)
    means = []
    for gi, w in enumerate(POOL_WINDOWS):
        lo = jnp.clip(t - w // 2, 0, T)
        hi = jnp.clip(t + w // 2, 0, T)
        csg = cs[:, :, gi]
        means.append((csg[:, hi] - csg[:, lo]) / (hi - lo).astype(F32)[None, :, None])
    y = jnp.stack(means, axis=2).astype(p.dtype) - xg
    y = jnp.einsum('btgd,gde->btge', y, pool_w).reshape(B_, T, GROUP_W)
    return y * pool_scale


def conformer_conv(p, cv_w, cv_b, ln_g, ln_b):
    a, g = jnp.split(p, 2, axis=-1)
    h = depthwise_conv(a * jax.nn.sigmoid(g), cv_w) + cv_b
    return jax.nn.silu(layernorm(h, ln_g, ln_b))


def mixers_out(p, o_dn, dn_norm_g, sg_ln_g, sg_ln_b, sg_w, sg_b, pool_w, pool_scale,
               cv_w, cv_b, cv_ln_g, cv_ln_b, w_out):
    off = DN_STATE_COLS
    z = p[..., off:off + Z_COLS]
    off += Z_COLS
    p_sg = p[..., off:off + SG_COLS]
    off += SG_COLS
    p_pool = p[..., off:off + POOL_COLS]
    off += POOL_COLS
    p_cv = p[..., off:off + CV_COLS]
    y = jnp.concatenate([
        gated_head_norm(o_dn, z, dn_norm_g),
        spatial_gating(p_sg, sg_ln_g, sg_ln_b, sg_w, sg_b),
        multiscale_pool(p_pool, pool_w, pool_scale),
        conformer_conv(p_cv, cv_w, cv_b, cv_ln_g, cv_ln_b),
    ], axis=-1)
    return y @ w_out


def swiglu(h, w1, w3, w2):
    return (jax.nn.silu(h @ w1) * (h @ w3)) @ w2


def moe_swiglu(h, router_w, router_b, w1, w3, w2):
    logits = (h @ router_w).astype(F32) + router_b.astype(F32)
    top_val, top_idx = lax.top_k(logits, TOP_K)
    gates = jax.nn.softmax(top_val, axis=-1)
    comb = jnp.sum(jax.nn.one_hot(top_idx, N_EXPERTS, dtype=F32) * gates[..., None], axis=-2)
    comb = comb.astype(h.dtype)
    out = jnp.zeros_like(h)
    for e in range(N_EXPERTS):
        out = out + comb[..., e:e + 1] * swiglu(h, w1[e], w3[e], w2[e])
    return out


def setup_inputs(seed: int = 0) -> dict:
    key = jax.random.key(seed)
    ks = iter(jax.random.split(key, 40))
    nrm = lambda shape, s: jax.random.normal(next(ks), shape, F32) * s
    gain = lambda shape: 1.0 + nrm(shape, 0.02)
    D, L = D_MODEL, DEPTH
    x = nrm((BATCH, SEQ, D), 1.0)
    c = nrm((BATCH, D), 1.0)
    ctx = nrm((BATCH, CTX_LEN, D), 1.0)
    c_ctx = nrm((D,), 1.0)
    ada_w = nrm((L, D, 6 * D), 0.5 * D ** -0.5)
    ada_b = nrm((L, 6 * D), 0.01)
    norm_mix_g = gain((L, D))
    w_in = nrm((L, D, IN_WIDTH), D ** -0.5)
    dn_conv_w = nrm((L, SHORT_CONV, QKV_COLS), SHORT_CONV ** -0.5)
    dn_a_log = jnp.log(jax.random.uniform(next(ks), (L, 2, DN_HEADS), F32, 1.0, 16.0))
    dt = jnp.exp(jax.random.uniform(next(ks), (L, 2, DN_HEADS), F32, math.log(1e-3), math.log(1e-1)))
    dn_dt_bias = dt + jnp.log(-jnp.expm1(-dt))
    dn_norm_g = gain((L, DN_HEAD_DIM))
    sg_ln_g = gain((L, GROUP_W))
    sg_ln_b = nrm((L, GROUP_W), 0.01)
    sg_w = nrm((L, SG_HEADS, SG_CHUNK, SG_CHUNK), SG_CHUNK ** -0.5)
    sg_b = 1.0 + nrm((L, SG_HEADS, SG_CHUNK), 0.02)
    pool_w = nrm((L, len(POOL_WINDOWS), POOL_GROUP_DIM, POOL_GROUP_DIM), POOL_GROUP_DIM ** -0.5)
    pool_scale = gain((L, GROUP_W))
    cv_w = nrm((L, CONF_CONV, GROUP_W), CONF_CONV ** -0.5)
    cv_b = nrm((L, GROUP_W), 0.01)
    cv_ln_g = gain((L, GROUP_W))
    cv_ln_b = nrm((L, GROUP_W), 0.01)
    w_out = nrm((L, MIX_WIDTH, D), MIX_WIDTH ** -0.5)
    norm_ffn_g = gain((L, D))
    ffn_w1 = nrm((N_DENSE, D, D_FF), D ** -0.5)
    ffn_w3 = nrm((N_DENSE, D, D_FF), D ** -0.5)
    ffn_w2 = nrm((N_DENSE, D_FF, D), D_FF ** -0.5)
    router_w = nrm((N_MOE, D, N_EXPERTS), D ** -0.5)
    router_b = nrm((N_MOE, N_EXPERTS), 0.01)
    moe_w1 = nrm((N_MOE, N_EXPERTS, D, D_FF_EXPERT), D ** -0.5)
    moe_w3 = nrm((N_MOE, N_EXPERTS, D, D_FF_EXPERT), D ** -0.5)
    moe_w2 = nrm((N_MOE, N_EXPERTS, D_FF_EXPERT, D), D_FF_EXPERT ** -0.5)
    final_norm_g = gain((D,))
    return {'x': x, 'c': c, 'ctx': ctx, 'c_ctx': c_ctx, 'ada_w': ada_w, 'ada_b': ada_b,
            'norm_mix_g': norm_mix_g, 'w_in': w_in, 'dn_conv_w': dn_conv_w, 'dn_a_log': dn_a_log,
            'dn_dt_bias': dn_dt_bias, 'dn_norm_g': dn_norm_g, 'sg_ln_g': sg_ln_g, 'sg_ln_b': sg_ln_b,
            'sg_w': sg_w, 'sg_b': sg_b, 'pool_w': pool_w, 'pool_scale': pool_scale, 'cv_w': cv_w,
            'cv_b': cv_b, 'cv_ln_g': cv_ln_g, 'cv_ln_b': cv_ln_b, 'w_out': w_out,
            'norm_ffn_g': norm_ffn_g, 'ffn_w1': ffn_w1, 'ffn_w3': ffn_w3, 'ffn_w2': ffn_w2,
            'router_w': router_w, 'router_b': router_b, 'moe_w1': moe_w1, 'moe_w3': moe_w3,
            'moe_w2': moe_w2, 'final_norm_g': final_norm_g}


def reference(x, c, ctx, c_ctx, ada_w, ada_b, norm_mix_g, w_in, dn_conv_w, dn_a_log, dn_dt_bias,
              dn_norm_g, sg_ln_g, sg_ln_b, sg_w, sg_b, pool_w, pool_scale, cv_w, cv_b, cv_ln_g,
              cv_ln_b, w_out, norm_ffn_g, ffn_w1, ffn_w3, ffn_w2, router_w, router_b, moe_w1,
              moe_w3, moe_w2, final_norm_g):
    B_, T, D = x.shape
    ROWS = T // GRID_W
    x = x + grid_pos_embed(ROWS, D).astype(x.dtype)[None]
    h_ctx = ctx
    s_c = jax.nn.silu(c)
    s_cc = jax.nn.silu(c_ctx)
    zero_state = jnp.zeros((B_, DN_HEADS, DN_HEAD_DIM, DN_HEAD_DIM), F32)
    for l in range(DEPTH):
        last = l == DEPTH - 1
        mod_x = (s_c @ ada_w[l] + ada_b[l])[:, None, :]
        mod_c = (s_cc @ ada_w[l] + ada_b[l])[None, None, :]
        shm, scm, gm, shf, scf, gf = jnp.split(mod_x, 6, axis=-1)
        cshm, cscm, cgm, cshf, cscf, cgf = jnp.split(mod_c, 6, axis=-1)
        dn_args = (dn_conv_w[l], dn_a_log[l], dn_dt_bias[l])
        bcd_args = (dn_norm_g[l], sg_ln_g[l], sg_ln_b[l], sg_w[l], sg_b[l], pool_w[l], pool_scale[l],
                    cv_w[l], cv_b[l], cv_ln_g[l], cv_ln_b[l], w_out[l])

        nc = modulate(rmsnorm(h_ctx, norm_mix_g[l]), cshm, cscm)
        pc = nc @ (w_in[l][:, :DN_STATE_COLS] if last else w_in[l])
        o_c, s_f, s_b = dn_bidir_scan(*dn_prepare(pc, *dn_args), zero_state, zero_state)
        nx = modulate(rmsnorm(x, norm_mix_g[l]), shm, scm)
        px = nx @ w_in[l]
        o_x, _, _ = dn_bidir_scan(*dn_prepare(px, *dn_args), s_f, s_b)
        x = x + gm * mixers_out(px, o_x, *bcd_args)
        if not last:
            h_ctx = h_ctx + cgm * mixers_out(pc, o_c, *bcd_args)

        if l % 2 == 0:
            i = l // 2
            ffn = lambda h: swiglu(h, ffn_w1[i], ffn_w3[i], ffn_w2[i])
        else:
            i = l // 2
            ffn = lambda h: moe_swiglu(h, router_w[i], router_b[i], moe_w1[i], moe_w3[i], moe_w2[i])
        x = x + gf * ffn(modulate(rmsnorm(x, norm_ffn_g[l]), shf, scf))
        if not last:
            h_ctx = h_ctx + cgf * ffn(modulate(rmsnorm(h_ctx, norm_ffn_g[l]), cshf, cscf))
    return rmsnorm(x, final_norm_g)
```

```python
from contextlib import ExitStack
import math
import numpy as np
import concourse.bass as bass
import concourse.mybir as mybir
from concourse.bass_utils import run_bass_kernel_spmd

F32 = mybir.dt.float32
BF16 = mybir.dt.bfloat16
AF = mybir.ActivationFunctionType
ALU = mybir.AluOpType
AX = mybir.AxisListType

D = 2048
T = 2048
TC = 256
TT = T + TC
NCH = 16
EPS = 1e-6
GW = 512
QKV = 1536
DNS = 1552
INW = 4624
OFF_Z = 1552
OFF_SGU = 2064
OFF_SGV = 2576
OFF_POOL = 3088
OFF_CVA = 3600
OFF_CVG = 4112
DFF = 5632
DFFE = 7168
NE = 8
PADL = 16
XOFF = PADL
COFF = PADL + T + 2 * PADL
PW = COFF + TC + PADL
BLOCKS = [(0, 512), (512, 512), (1024, 512), (1536, 512), (2048, 256)]
NTILE = TT // 128


def pcol(c0):
    return XOFF + c0 if c0 < T else COFF + (c0 - T)


class Buf:
    __slots__ = ("w", "r", "excl")

    def __init__(self, excl=False):
        self.w = None
        self.r = {}
        self.excl = excl


class Sched:
    ENG = ("pe", "act", "dve", "pool", "sp")

    def __init__(self, nc, es, n_dma_sems=24):
        self.nc = nc
        self.eng = {"pe": nc.tensor, "act": nc.scalar, "dve": nc.vector, "pool": nc.gpsimd, "sp": nc.sync}
        self.sem = {k: es.enter_context(nc.semaphore("s_" + k)) for k in ("pe", "act", "dve", "pool")}
        self.cnt = {k: 0 for k in ("pe", "act", "dve", "pool")}
        self.n_dma = n_dma_sems
        self.dsem = [es.enter_context(nc.semaphore("s_dma%d" % i)) for i in range(n_dma_sems)]
        self.dcnt = [0] * n_dma_sems
        self.dnext = 0
        self.dnext_sw = 0
        self.n_sw = 8
        self.known = {e: {} for e in self.ENG}
        self.nops = 0
        self.out_toks = []

    def _semh(self, key):
        return self.dsem[key[1]] if isinstance(key, tuple) else self.sem[key]

    def _wait(self, e, tok):
        if tok is None:
            return
        key, val = tok
        kn = self.known[e]
        if kn.get(key, 0) >= val:
            return
        self.eng[e].wait_ge(self._semh(key), val)
        kn[key] = val

    def _deps(self, e, reads, writes):
        for b in reads:
            if b.w is not None:
                self._wait(e, b.w)
            if b.excl:
                for re_, tok in b.r.items():
                    if re_ != e:
                        self._wait(e, tok)
        for b in writes:
            if b.w is not None and b.w[0] != e:
                self._wait(e, b.w)
            for re_, tok in b.r.items():
                if re_ != e:
                    self._wait(e, tok)

    def op(self, e, fn, reads=(), writes=(), inc=True):
        self._deps(e, reads, writes)
        ins = fn(self.eng[e])
        self.nops += 1
        if inc:
            self.cnt[e] += 1
            ins.then_inc(self.sem[e], 1)
            tok = (e, self.cnt[e])
        else:
            tok = (e, self.cnt[e] + 1)
        for b in reads:
            b.r[e] = tok
        for b in writes:
            b.w = tok
            b.r = {}
        return ins

    def dma(self, q, out, in_, reads=(), writes=(), **kw):
        if q == "pool":
            i = self.dnext_sw
            self.dnext_sw = (self.dnext_sw + 1) % self.n_sw
        else:
            i = self.n_sw + self.dnext
            self.dnext = (self.dnext + 1) % (self.n_dma - self.n_sw)
        if self.dcnt[i] > 0:
            self._wait(q, (("dma", i), 16 * self.dcnt[i]))
        self._deps(q, reads, writes)
        ins = self.eng[q].dma_start(out=out, in_=in_, **kw)
        self.dcnt[i] += 1
        ins.then_inc(self.dsem[i], 16)
        tok = (("dma", i), 16 * self.dcnt[i])
        for b in reads:
            b.r[("dma", i)] = tok
        for b in writes:
            b.w = tok
            b.r = {}
        self.nops += 1
        return tok

    def barrier(self):
        for e in self.ENG:
            for f in ("pe", "act", "dve", "pool"):
                if self.cnt[f] > 0:
                    self._wait(e, (f, self.cnt[f]))
            for i in range(self.n_dma):
                if self.dcnt[i] > 0:
                    self._wait(e, (("dma", i), 16 * self.dcnt[i]))


class Rot:
    uid = [0]

    def __init__(self, nc, es, name, shape, dt, n):
        Rot.uid[0] += 1
        self.t = [es.enter_context(nc.sbuf_tensor("rt_%s_%d_%d" % (name, Rot.uid[0], i), shape, dt)) for i in range(n)]
        self.b = [Buf() for _ in range(n)]
        self.i = 0
        self.n = n

    def next(self):
        i = self.i
        self.i = (i + 1) % self.n
        return self.t[i], self.b[i]


def build_nc(debug=False, stop_after=None, layers=2, dffe=7168):
    nc = bass.Bass("TRN2", target_bir_lowering=False)
    inp = {}

    def IN(name, shape):
        inp[name] = nc.dram_tensor(name, list(shape), F32, kind="ExternalInput").ap()
        return inp[name]

    xT_in = IN("xT", [D, T]); ctxT_in = IN("ctxT", [D, TC]); posT_in = IN("posT", [D, T])
    c2_in = IN("c2", [128, NCH, 2]); hmask_in = IN("hmask", [128, 2])
    ada_w = IN("ada_w", [2, D, 6 * D]); ada_bT = IN("ada_bT", [128, 2, 96])
    nmg_in = IN("norm_mix_gT", [128, 2, NCH]); nfg_in = IN("norm_ffn_gT", [128, 2, NCH]); fng_in = IN("final_norm_gT", [128, NCH])
    w_in = IN("w_in", [2, D, INW]); w_out = IN("w_out", [2, D, D])
    dncw_in = IN("dn_conv_wT", [128, 2, 12, 5]); alog_in = IN("dn_a_log_r", [128, 2, 8]); dtb_in = IN("dn_dt_bias_r", [128, 2, 8])
    dng_in = IN("dn_norm_gT", [128, 2]); sglg_in = IN("sg_ln_g_r", [2, 128, GW]); sglb_in = IN("sg_ln_b_r", [2, 128, GW])
    sgwT_in = IN("sg_wT", [2, 128, 4, 128]); sgb_in = IN("sg_b_r", [2, 128, 4, 128])
    poolw_in = IN("pool_wT", [2, 128, 4, 128]); pools_in = IN("pool_scaleT", [128, 2, 4])
    cvw_in = IN("cv_wT", [128, 2, 4, 31]); cvb_in = IN("cv_bT", [128, 2, 4]); cvlg_in = IN("cv_ln_gT", [128, 2, 4]); cvlb_in = IN("cv_ln_bT", [128, 2, 4])
    if stop_after in (None, "F0"):
        ffn_w1 = IN("ffn_w1", [1, D, DFF]); ffn_w3 = IN("ffn_w3", [1, D, DFF]); ffn_w2 = IN("ffn_w2", [1, DFF, D])
    else:
        ffn_w1 = IN("ffn_w1", [1, 128, 256]); ffn_w3 = IN("ffn_w3", [1, 128, 256]); ffn_w2 = IN("ffn_w2", [1, 256, 128])
    router_w = IN("router_w", [1, D, NE]); routerb_in = IN("router_b_r", [128, NE])
    if stop_after in (None, "MOE"):
        moe_w1 = IN("moe_w1", [1, NE, D, dffe]); moe_w3 = IN("moe_w3", [1, NE, D, dffe]); moe_w2 = IN("moe_w2", [1, NE, dffe, D])
    else:
        moe_w1 = IN("moe_w1", [1, 1, 128, 256]); moe_w3 = IN("moe_w3", [1, 1, 128, 256]); moe_w2 = IN("moe_w2", [1, 1, 256, 128])
    masks_in = IN("masks", [128, 12, 4, 128])
    invcnt_in = IN("invcnt", [4, 128, PW]); sel8_in = IN("sel8", [8, NE, 128])

    outT = nc.dram_tensor("outT", [D, T // 2], F32, kind="ExternalOutput").ap()
    skind = "ExternalOutput" if debug else "Internal"
    XT = nc.dram_tensor("XT", [D, TT], F32, kind=skind).ap()
    YT = nc.dram_tensor("YT", [D, TT], BF16, kind=skind).ap()
    QKVT = nc.dram_tensor("QKVT", [QKV, TT], BF16, kind=skind).ap()
    ZT = nc.dram_tensor("ZT", [GW, TT], BF16, kind=skind).ap()
    OSC = nc.dram_tensor("OSC", [2, GW, TT], F32, kind=skind).ap()
    DBG = nc.dram_tensor("DBG", [128, 2, 96, 2], F32, kind=skind).ap() if debug else None
    LAD = nc.dram_tensor("LAD", [128, NTILE, 16], F32, kind=skind).ap() if debug else None

    with ExitStack() as es:
        S = Sched(nc, es)
        _uid = [0]

        def SB(st, name, shape, dt=F32):
            _uid[0] += 1
            return st.enter_context(nc.sbuf_tensor("sb_%s_%d" % (name, _uid[0]), list(shape), dt))
        psf = [es.enter_context(nc.psum_tensor("psf%d" % i, [128, 512], F32)) for i in range(6)]
        psfb = [Buf(excl=True) for _ in range(6)]
        psh = [es.enter_context(nc.psum_tensor("psh%d" % i, [128, 1024], BF16)) for i in range(2)]
        pshb = [Buf(excl=True) for _ in range(2)]
        pctr = [0, 0]

        def PS():
            i = pctr[0] % 6
            pctr[0] += 1
            return psf[i], psfb[i]

        def PSH():
            i = pctr[1] % 2
            pctr[1] += 1
            return psh[i], pshb[i]

        ident32t = SB(es, "ident32", [128, 128]); idb = Buf()
        S.dma("sp", ident32t[:], masks_in[:, 0, 0, :], writes=[idb])
        identht = SB(es, "identh", [128, 128], BF16); mkhb = Buf()
        S.op("dve", lambda e: e.tensor_copy(out=identht[:], in_=ident32t[:]), reads=[idb], writes=[mkhb])
        ident32 = ident32t[:]
        identh = identht[:]
        ones32 = SB(es, "ones32", [128, 128]); ones16 = SB(es, "ones16", [128, 128], BF16); onb = Buf()
        S.op("dve", lambda e: e.memset(ones32[:], 1.0), writes=[onb])
        S.op("dve", lambda e: e.memset(ones16[:], 1.0), writes=[onb])
        MOD = SB(es, "MOD", [128, 2, 96, 2]); modb = Buf()
        GS = SB(es, "GS", [128, 2, 2, NCH, 2]); gsb = Buf()
        hmask = SB(es, "hmask", [128, 2]); hmb = Buf()
        S.dma("sp", hmask[:], hmask_in, writes=[hmb])
        small = {}
        for nm, src, shp in (("nmg", nmg_in, [128, 2, NCH]), ("nfg", nfg_in, [128, 2, NCH]), ("fng", fng_in, [128, NCH]),
                             ("dncw", dncw_in, [128, 2, 12, 5]), ("alog", alog_in, [128, 2, 8]), ("dtb", dtb_in, [128, 2, 8]),
                             ("dng", dng_in, [128, 2]), ("pools", pools_in, [128, 2, 4]), ("cvw", cvw_in, [128, 2, 4, 31]),
                             ("cvb", cvb_in, [128, 2, 4]), ("cvlg", cvlg_in, [128, 2, 4]), ("cvlb", cvlb_in, [128, 2, 4]),
                             ("adab", ada_bT, [128, 2, 96]), ("rb", routerb_in, [128, NE])):
            t = SB(es, "c_" + nm, shp)
            b = Buf()
            S.dma("sp", t[:], src, writes=[b])
            small[nm] = (t, b)
        negA = SB(es, "negA", [128, 2, 8]); negAb = Buf()
        S.op("act", lambda e: e.activation(out=negA[:], in_=small["alog"][0][:], func=AF.Exp), reads=[small["alog"][1]], writes=[negAb])
        S.op("dve", lambda e: e.tensor_scalar(out=negA[:], in0=negA[:], scalar1=-1.0, scalar2=None, op0=ALU.mult), reads=[negAb], writes=[negAb])

        def mcol(l, v, c, w):
            return MOD[:, l, v * NCH + c, w:w + 1]

        with ExitStack() as st:
            c2 = SB(st, "c2", [128, NCH, 2]); c2b = Buf()
            S.dma("sp", c2[:], c2_in, writes=[c2b])
            s2 = SB(st, "s2", [128, NCH, 2]); s2b = Buf()
            S.op("act", lambda e: e.activation(out=s2[:], in_=c2[:], func=AF.Silu), reads=[c2b], writes=[s2b])
            arot = Rot(nc, st, "adaw", [128, NCH, 512], F32, 2)
            for l in range(layers):
                for sl in range(24):
                    wt, wb = arot.next()
                    S.dma("sp", wt[:], ada_w[l, :, sl * 512:(sl + 1) * 512].rearrange("(c p) n -> p c n", p=128), writes=[wb])
                    ps, pb = PS()
                    for j in range(4):
                        for k in range(NCH):
                            S.op("pe", lambda e: e.matmul(ps[:, j * 2:j * 2 + 2], lhsT=wt[:, k, j * 128:(j + 1) * 128], rhs=s2[:, k, :],
                                                          start=(k == 0), stop=(k == NCH - 1)),
                                 reads=[wb, s2b], writes=[pb], inc=(k == NCH - 1 and j == 3))
                    S.op("dve", lambda e: e.tensor_tensor(out=MOD[:, l, sl * 4:(sl + 1) * 4, :],
                                                          in0=ps[:, 0:8].rearrange("p (j w) -> p j w", w=2),
                                                          in1=small["adab"][0][:, l, sl * 4:(sl + 1) * 4].unsqueeze(2).to_broadcast([128, 4, 2]),
                                                          op=ALU.add),
                         reads=[pb, small["adab"][1]], writes=[modb])
            for l in range(layers):
                for fi, (gname, v) in enumerate((("nmg", 1), ("nfg", 4))):
                    S.op("dve", lambda e: e.scalar_tensor_tensor(out=GS[:, l, fi, :, :], in0=MOD[:, l, v * NCH:(v + 1) * NCH, :], scalar=1.0,
                                                                 in1=small[gname][0][:, l, :].unsqueeze(2).to_broadcast([128, NCH, 2]),
                                                                 op0=ALU.add, op1=ALU.mult),
                         reads=[modb, small[gname][1]], writes=[gsb])
            if debug:
                S.dma("sp", DBG, MOD[:], reads=[modb])
            xrot = Rot(nc, st, "xin", [128, 2048], F32, 2)
            prot = Rot(nc, st, "pin", [128, 2048], F32, 2)
            for c in range(NCH):
                xt, xb = xrot.next()
                pt, pb2 = prot.next()
                S.dma("sp", xt[:], xT_in[c * 128:(c + 1) * 128, :], writes=[xb])
                S.dma("sp", pt[:], posT_in[c * 128:(c + 1) * 128, :], writes=[pb2])
                S.op("dve", lambda e: e.tensor_tensor(out=xt[:], in0=xt[:], in1=pt[:], op=ALU.add), reads=[xb, pb2], writes=[xb])
                S.dma("sp", XT[c * 128:(c + 1) * 128, 0:T], xt[:], reads=[xb])
            crot = Rot(nc, st, "cin", [128, NCH, TC], F32, 1)
            ct, cb = crot.next()
            S.dma("sp", ct[:], ctxT_in.rearrange("(c p) t -> p c t", p=128), writes=[cb])
            S.dma("sp", XT[:, T:TT].rearrange("(c p) t -> p c t", p=128), ct[:], reads=[cb])
            S.barrier()
        xtb = Buf()

        def norm_mod(st, l, fi, srcs, R, Rb, hp_h32=None):
            pass

        def rms_block(l, fi, w, xt, xb, n, R, Rb, dcol, scr, hp=False, h32=None):
            sqt, sqb, rst, rsb, tmt, tmb = scr
            if hp:
                S.op("act", lambda e: e.activation(out=sqt[:, :, 0:n], in_=xt, func=AF.Square), reads=[xb], writes=[sqb])
            else:
                S.op("act", lambda e: e.activation(out=sqt[:, :, 0:n], in_=xt, func=AF.Square), reads=[xb], writes=[sqb])
            ps, pb = PS()
            on = ones32 if sqt.dtype == F32 else ones16
            for k in range(NCH):
                S.op("pe", lambda e: e.matmul(ps[:, 0:n], lhsT=on[:], rhs=sqt[:, k, 0:n], start=(k == 0), stop=(k == NCH - 1)),
                     reads=[sqb, onb], writes=[pb], inc=(k == NCH - 1))
            S.op("act", lambda e: e.activation(out=rst[:, 0:n], in_=ps[:, 0:n], func=AF.Sqrt, bias=EPS, scale=1.0 / D), reads=[pb], writes=[rsb])
            S.op("dve", lambda e: e.reciprocal(out=rst[:, 0:n], in_=rst[:, 0:n]), reads=[rsb], writes=[rsb])
            S.op("dve", lambda e: e.tensor_tensor(out=tmt[:, :, 0:n], in0=xt, in1=rst[:, 0:n].unsqueeze(1).to_broadcast([128, NCH, n]), op=ALU.mult),
                 reads=[xb, rsb], writes=[tmb])
            sv = 0 if fi == 0 else 3
            for k in range(NCH):
                S.op("act", lambda e: e.activation(out=R[:, k, dcol:dcol + n], in_=tmt[:, k, 0:n], func=AF.Identity,
                                                   bias=mcol(l, sv, k, w), scale=GS[:, l, fi, k, w:w + 1]),
                     reads=[tmb, modb, gsb], writes=[Rb])
                if h32 is not None:
                    S.op("act", lambda e: e.activation(out=h32[0][:, k, dcol:dcol + n], in_=tmt[:, k, 0:n], func=AF.Identity,
                                                       bias=mcol(l, sv, k, w), scale=GS[:, l, fi, k, w:w + 1]),
                         reads=[tmb, modb, gsb], writes=[h32[1]])

        def mixing_layer(l):
            last = (l == layers - 1) and (layers == 2)
            with ExitStack() as st:
                R = SB(st, "R", [128, NCH, TT], BF16); Rb = Buf()
                LA = SB(st, "LA", [128, NTILE, 8]); BETA = SB(st, "BETA", [128, NTILE, 8]); NBETA = SB(st, "NBETA", [128, NTILE, 8]); lab = Buf()
                with ExitStack() as sa:
                    xrot = Rot(nc, sa, "xa", [128, NCH, 256], F32, 2)
                    sqt = SB(sa, "sqa", [128, NCH, 256], BF16); rst = SB(sa, "rsa", [128, 256]); tmt = SB(sa, "tma", [128, NCH, 256])
                    scr = (sqt, Buf(), rst, Buf(), tmt, Buf())
                    for (c0, n) in [(i * 256, 256) for i in range(TT // 256)]:
                        xt, xb = xrot.next()
                        S.dma("sp", xt[:, :, 0:n], XT[:, c0:c0 + n].rearrange("(c p) t -> p c t", p=128), reads=[xtb], writes=[xb])
                        rms_block(l, 0, 0 if c0 < T else 1, xt[:, :, 0:n], xb, n, R, Rb, c0, scr)
                    S.barrier()
                if stop_after == "A":
                    S.dma("sp", YT.rearrange("(c p) t -> p c t", p=128), R[:], reads=[Rb])
                    S.barrier()
                    return
                wrot = Rot(nc, st, "wsl", [128, NCH, 256], BF16, 3)

                def proj_fm(col0, ncols, evac, blocks=BLOCKS):
                    wt, wb = wrot.next()
                    S.dma("pool", wt[:, :, 0:ncols], w_in[l, :, col0:col0 + ncols].rearrange("(c p) n -> p c n", p=128), writes=[wb])
                    for j in range(ncols // 128):
                        for bi, (c0, n) in enumerate(blocks):
                            ps, pb = PS()
                            for k in range(NCH):
                                S.op("pe", lambda e: e.matmul(ps[:, 0:n], lhsT=wt[:, k, j * 128:(j + 1) * 128], rhs=R[:, k, c0:c0 + n],
                                                              start=(k == 0), stop=(k == NCH - 1)),
                                     reads=[wb, Rb], writes=[pb], inc=(k == NCH - 1))
                            evac(j, c0, n, ps, pb)

                with ExitStack() as sq_:
                    pq = SB(sq_, "pq", [128, PW], BF16); pqb = Buf()
                    S.op("pool", lambda e: e.memset(pq[:], 0.0), writes=[pqb])
                    dg = SB(sq_, "dg5", [128, 5, 128], BF16); dgb = Buf()
                    srot = Rot(nc, sq_, "sil", [128, 512], F32, 2)
                    s2rot = Rot(nc, sq_, "sil2", [128, 512], F32, 2)
                    rrot = Rot(nc, sq_, "rr", [128, 512], F32, 2)
                    orot = Rot(nc, sq_, "qo", [128, 512], BF16, 3)
                    for sl in range(6):
                        def evac_qkv(j, c0, n, ps, pb, sl=sl):
                            S.op("act", lambda e: e.copy(out=pq[:, pcol(c0):pcol(c0) + n], in_=ps[:, 0:n]), reads=[pb], writes=[pqb])
                            if c0 + n < TT:
                                return
                            ch = sl * 2 + j
                            for k in range(5):
                                S.op("pool", lambda e: e.tensor_scalar(out=dg[:, k, :], in0=identh, scalar1=small["dncw"][0][:, l, ch, k:k + 1],
                                                                       scalar2=None, op0=ALU.mult),
                                     reads=[mkhb, small["dncw"][1]], writes=[dgb])
                            for (b0, bn) in BLOCKS:
                                ps2, pb2 = PS()
                                for k in range(5):
                                    S.op("pe", lambda e: e.matmul(ps2[:, 0:bn], lhsT=dg[:, k, :], rhs=pq[:, pcol(b0) + k - 2:pcol(b0) + k - 2 + bn],
                                                                  start=(k == 0), stop=(k == 4)),
                                         reads=[dgb, pqb], writes=[pb2], inc=(k == 4))
                                ot, ob = orot.next()
                                if ch < 8:
                                    s_, sb_ = srot.next()
                                    S.op("act", lambda e: e.activation(out=s_[:, 0:bn], in_=ps2[:, 0:bn], func=AF.Silu), reads=[pb2], writes=[sb_])
                                    q2, q2b = s2rot.next()
                                    S.op("dve", lambda e: e.tensor_tensor(out=q2[:, 0:bn], in0=s_[:, 0:bn], in1=s_[:, 0:bn], op=ALU.mult), reads=[sb_], writes=[q2b])
                                    ps3, pb3 = PS()
                                    S.op("pe", lambda e: e.matmul(ps3[:, 0:bn], lhsT=ones32[:], rhs=q2[:, 0:bn], start=True, stop=True),
                                         reads=[q2b, onb], writes=[pb3])
                                    r_, rb_ = rrot.next()
                                    S.op("act", lambda e: e.activation(out=r_[:, 0:bn], in_=ps3[:, 0:bn], func=AF.Sqrt, bias=EPS, scale=1.0), reads=[pb3], writes=[rb_])
                                    S.op("dve", lambda e: e.reciprocal(out=r_[:, 0:bn], in_=r_[:, 0:bn]), reads=[rb_], writes=[rb_])
                                    sc = (128.0 ** -0.5) if ch < 4 else 1.0
                                    S.op("dve", lambda e: e.scalar_tensor_tensor(out=ot[:, 0:bn], in0=s_[:, 0:bn], scalar=sc, in1=r_[:, 0:bn],
                                                                                 op0=ALU.mult, op1=ALU.mult), reads=[sb_, rb_], writes=[ob])
                                else:
                                    S.op("act", lambda e: e.activation(out=ot[:, 0:bn], in_=ps2[:, 0:bn], func=AF.Silu), reads=[pb2], writes=[ob])
                                S.dma("sp", QKVT[ch * 128:(ch + 1) * 128, b0:b0 + bn], ot[:, 0:bn], reads=[ob])
                        proj_fm(sl * 256, 256, evac_qkv)
                    S.barrier()
                with ExitStack() as sg_:
                    wab = SB(sg_, "wab", [128, NCH, 16], BF16); wabb = Buf()
                    S.dma("pool", wab[:], w_in[l, :, QKV:DNS].rearrange("(c p) n -> p c n", p=128), writes=[wabb])
                    xa = SB(sg_, "xa_", [128, 8]); xab = Buf()
                    for n_ in range(NTILE):
                        ps, pb = PS()
                        for k in range(NCH):
                            S.op("pe", lambda e: e.matmul(ps[:, 0:16], lhsT=R[:, k, n_ * 128:(n_ + 1) * 128], rhs=wab[:, k, :],
                                                          start=(k == 0), stop=(k == NCH - 1)), reads=[Rb, wabb], writes=[pb], inc=(k == NCH - 1))
                        S.op("dve", lambda e: e.tensor_tensor(out=xa[:], in0=ps[:, 0:8], in1=small["dtb"][0][:, l, :], op=ALU.add),
                             reads=[pb, small["dtb"][1]], writes=[xab])
                        S.op("dve", lambda e: e.tensor_scalar(out=xa[:], in0=xa[:], scalar1=30.0, scalar2=None, op0=ALU.min), reads=[xab], writes=[xab])
                        S.op("act", lambda e: e.activation(out=xa[:], in_=xa[:], func=AF.Exp), reads=[xab], writes=[xab])
                        S.op("act", lambda e: e.activation(out=xa[:], in_=xa[:], func=AF.Ln, bias=1.0), reads=[xab], writes=[xab])
                        S.op("dve", lambda e: e.tensor_tensor(out=LA[:, n_, :], in0=xa[:], in1=negA[:, l, :], op=ALU.mult), reads=[xab, negAb], writes=[lab])
                        S.op("act", lambda e: e.activation(out=BETA[:, n_, :], in_=ps[:, 8:16], func=AF.Sigmoid), reads=[pb], writes=[lab])
                        S.op("dve", lambda e: e.tensor_scalar(out=NBETA[:, n_, :], in0=BETA[:, n_, :], scalar1=-1.0, scalar2=None, op0=ALU.mult),
                             reads=[lab], writes=[lab])
                    if debug:
                        S.dma("sp", LAD[:, :, 0:8], LA[:], reads=[lab])
                        S.dma("sp", LAD[:, :, 8:16], BETA[:], reads=[lab])
                    S.barrier()
                with ExitStack() as sz_:
                    zrot = Rot(nc, sz_, "zo", [128, 512], BF16, 3)
                    for sl in range(2):
                        def evac_z(j, c0, n, ps, pb, sl=sl):
                            zt, zb = zrot.next()
                            S.op("act", lambda e: e.activation(out=zt[:, 0:n], in_=ps[:, 0:n], func=AF.Silu), reads=[pb], writes=[zb])
                            r0 = (sl * 2 + j) * 128
                            S.dma("sp", ZT[r0:r0 + 128, c0:c0 + n], zt[:, 0:n], reads=[zb])
                        proj_fm(OFF_Z + sl * 256, 256, evac_z)
                    S.barrier()
                with ExitStack() as sp_:
                    PP = SB(sp_, "PP", [128, PW]); ppb = Buf()
                    W1 = SB(sp_, "PW1", [128, PW]); w1b = Buf()
                    W2 = SB(sp_, "PW2", [128, PW]); w2b = Buf()
                    YB = SB(sp_, "PYB", [128, PW], BF16); ybb = Buf()
                    ICN = SB(sp_, "ICN", [128, PW]); icb = Buf()
                    pwt = SB(sp_, "pwt", [128, 4, 128], BF16); pwb = Buf()
                    S.dma("pool", pwt[:], poolw_in[l], writes=[pwb])
                    S.op("pool", lambda e: e.memset(PP[:], 0.0), writes=[ppb])
                    S.op("pool", lambda e: e.memset(W1[:], 0.0), writes=[w1b])
                    S.op("pool", lambda e: e.memset(W2[:], 0.0), writes=[w2b])
                    yrot = Rot(nc, sp_, "pyo", [128, 512], BF16, 3)
                    for sl in range(2):
                        def evac_p(j, c0, n, ps, pb, sl=sl):
                            S.op("act", lambda e: e.copy(out=PP[:, pcol(c0):pcol(c0) + n], in_=ps[:, 0:n]), reads=[pb], writes=[ppb])
                            if c0 + n < TT:
                                return
                            g = sl * 2 + j
                            S.dma("sp", ICN[:], invcnt_in[g], writes=[icb])
                            lo, hi = 16, PW - 16
                            S.op("dve", lambda e: e.tensor_tensor(out=W1[:, 1:PW], in0=PP[:, 1:PW], in1=PP[:, 0:PW - 1], op=ALU.add), reads=[ppb], writes=[w1b])
                            cur, curb, oth, othb = W1, w1b, W2, w2b
                            for sh in ((1,), (1, 2), (1, 2, 4))[g - 1] if g > 0 else ():
                                S.op("dve", lambda e: e.tensor_tensor(out=oth[:, sh:PW - sh], in0=cur[:, 0:PW - 2 * sh], in1=cur[:, 2 * sh:PW], op=ALU.add),
                                     reads=[curb], writes=[othb])
                                cur, curb, oth, othb = oth, othb, cur, curb
                            S.op("dve", lambda e: e.tensor_tensor(out=oth[:, lo:hi], in0=cur[:, lo:hi], in1=ICN[:, lo:hi], op=ALU.mult), reads=[curb, icb], writes=[othb])
                            S.op("pool", lambda e: e.tensor_tensor(out=YB[:, lo:hi], in0=oth[:, lo:hi], in1=PP[:, lo:hi], op=ALU.subtract), reads=[othb, ppb], writes=[ybb])
                            for (b0, bn) in BLOCKS:
                                ps2, pb2 = PS()
                                S.op("pe", lambda e: e.matmul(ps2[:, 0:bn], lhsT=pwt[:, g, :], rhs=YB[:, pcol(b0):pcol(b0) + bn], start=True, stop=True),
                                     reads=[pwb, ybb], writes=[pb2])
                                yt, yb = yrot.next()
                                S.op("act", lambda e: e.activation(out=yt[:, 0:bn], in_=ps2[:, 0:bn], func=AF.Identity, scale=small["pools"][0][:, l, g:g + 1]),
                                     reads=[pb2, small["pools"][1]], writes=[yb])
                                S.dma("sp", YT[1024 + g * 128:1024 + (g + 1) * 128, b0:b0 + bn], yt[:, 0:bn], reads=[yb])
                        proj_fm(OFF_POOL + sl * 256, 256, evac_p)
                    S.barrier()
                with ExitStack() as sc_:
                    A_ = SB(sc_, "cvA", [128, 4, TT]); ab_ = Buf()
                    U_ = SB(sc_, "cvU", [128, 4, PW], BF16); ub_ = Buf()
                    H_ = A_; hb_ = ab_
                    S.op("pool", lambda e: e.memset(U_[:], 0.0), writes=[ub_])
                    dg31 = SB(sc_, "dg31", [128, 31, 128], BF16); d31b = Buf()
                    sgr = Rot(nc, sc_, "cvsg", [128, 512], F32, 2)
                    for sl in range(2):
                        def evac_a(j, c0, n, ps, pb, sl=sl):
                            S.op("act", lambda e: e.copy(out=A_[:, sl * 2 + j, c0:c0 + n], in_=ps[:, 0:n]), reads=[pb], writes=[ab_])
                        proj_fm(OFF_CVA + sl * 256, 256, evac_a)
                    for sl in range(2):
                        def evac_g(j, c0, n, ps, pb, sl=sl):
                            ch = sl * 2 + j
                            sg, sgb_ = sgr.next()
                            S.op("act", lambda e: e.activation(out=sg[:, 0:n], in_=ps[:, 0:n], func=AF.Sigmoid), reads=[pb], writes=[sgb_])
                            S.op("dve", lambda e: e.tensor_tensor(out=U_[:, ch, pcol(c0):pcol(c0) + n], in0=A_[:, ch, c0:c0 + n], in1=sg[:, 0:n], op=ALU.mult),
                                 reads=[ab_, sgb_], writes=[ub_])
                            if c0 + n < TT:
                                return
                            for k in range(31):
                                S.op("pool", lambda e: e.tensor_scalar(out=dg31[:, k, :], in0=identh, scalar1=small["cvw"][0][:, l, ch, k:k + 1],
                                                                       scalar2=None, op0=ALU.mult), reads=[mkhb, small["cvw"][1]], writes=[d31b])
                            for (b0, bn) in BLOCKS:
                                ps2, pb2 = PS()
                                for k in range(31):
                                    S.op("pe", lambda e: e.matmul(ps2[:, 0:bn], lhsT=dg31[:, k, :], rhs=U_[:, ch, pcol(b0) + k - 15:pcol(b0) + k - 15 + bn],
                                                                  start=(k == 0), stop=(k == 30)), reads=[d31b, ub_], writes=[pb2], inc=(k == 30))
                                S.op("act", lambda e: e.activation(out=H_[:, ch, b0:b0 + bn], in_=ps2[:, 0:bn], func=AF.Identity,
                                                                   bias=small["cvb"][0][:, l, ch:ch + 1], scale=1.0), reads=[pb2, small["cvb"][1]], writes=[hb_])
                        proj_fm(OFF_CVG + sl * 256, 256, evac_g)
                    hc = SB(sc_, "cvhc", [128, 4, 256]); hcb = Buf()
                    hq = SB(sc_, "cvhq", [128, 4, 256]); hqb = Buf()
                    rs_ = SB(sc_, "cvrs", [128, 256]); rsb_ = Buf()
                    cyr = Rot(nc, sc_, "cvy", [128, 4, 256], BF16, 2)
                    for (b0, bn) in [(i * 256, 256) for i in range(TT // 256)]:
                        ps, pb = PS()
                        for ch in range(4):
                            S.op("pe", lambda e: e.matmul(ps[:, 0:bn], lhsT=ones32[:], rhs=H_[:, ch, b0:b0 + bn], start=(ch == 0), stop=(ch == 3)),
                                 reads=[hb_, onb], writes=[pb], inc=(ch == 3))
                        S.op("act", lambda e: e.activation(out=rs_[:, 0:bn], in_=ps[:, 0:bn], func=AF.Identity, scale=-1.0 / GW), reads=[pb], writes=[rsb_])
                        S.op("dve", lambda e: e.tensor_tensor(out=hc[:, :, 0:bn], in0=H_[:, :, b0:b0 + bn], in1=rs_[:, 0:bn].unsqueeze(1).to_broadcast([128, 4, bn]), op=ALU.add),
                             reads=[hb_, rsb_], writes=[hcb])
                        S.op("act", lambda e: e.activation(out=hq[:, :, 0:bn], in_=hc[:, :, 0:bn], func=AF.Square), reads=[hcb], writes=[hqb])
                        ps2, pb2 = PS()
                        for ch in range(4):
                            S.op("pe", lambda e: e.matmul(ps2[:, 0:bn], lhsT=ones32[:], rhs=hq[:, ch, 0:bn], start=(ch == 0), stop=(ch == 3)),
                                 reads=[hqb, onb], writes=[pb2], inc=(ch == 3))
                        S.op("act", lambda e: e.activation(out=rs_[:, 0:bn], in_=ps2[:, 0:bn], func=AF.Sqrt, bias=EPS, scale=1.0 / GW), reads=[pb2], writes=[rsb_])
                        S.op("dve", lambda e: e.reciprocal(out=rs_[:, 0:bn], in_=rs_[:, 0:bn]), reads=[rsb_], writes=[rsb_])
                        S.op("dve", lambda e: e.tensor_tensor(out=hc[:, :, 0:bn], in0=hc[:, :, 0:bn], in1=rs_[:, 0:bn].unsqueeze(1).to_broadcast([128, 4, bn]), op=ALU.mult),
                             reads=[hcb, rsb_], writes=[hcb])
                        yt, yb = cyr.next()
                        for ch in range(4):
                            S.op("act", lambda e: e.activation(out=yt[:, ch, 0:bn], in_=hc[:, ch, 0:bn], func=AF.Silu,
                                                               bias=small["cvlb"][0][:, l, ch:ch + 1], scale=small["cvlg"][0][:, l, ch:ch + 1]),
                                 reads=[hcb, small["cvlb"][1], small["cvlg"][1]], writes=[yb])
                        S.dma("sp", YT[1536:2048, b0:b0 + bn].rearrange("(c p) t -> p c t", p=128), yt[:, :, 0:bn], reads=[yb])
                    S.barrier()
                with ExitStack() as ss_:
                    UT = SB(ss_, "sgU", [128, 4, TT], BF16); utb = Buf()
                    g1 = Rot(nc, ss_, "gl1", [128, 512], F32, 2)
                    g2 = Rot(nc, ss_, "gl2", [128, 512], F32, 2)
                    g3 = Rot(nc, ss_, "gl3", [128, 512], F32, 2)

                    def gelu(ps, pb, n, out_ap, outb, extra_reads=()):
                        x_, xb_ = g1.next()
                        S.op("act", lambda e: e.copy(out=x_[:, 0:n], in_=ps), reads=[pb], writes=[xb_])
                        t_, tb_ = g2.next()
                        S.op("dve", lambda e: e.tensor_tensor(out=t_[:, 0:n], in0=x_[:, 0:n], in1=x_[:, 0:n], op=ALU.mult), reads=[xb_], writes=[tb_])
                        S.op("dve", lambda e: e.tensor_scalar(out=t_[:, 0:n], in0=t_[:, 0:n], scalar1=0.044715, scalar2=1.0, op0=ALU.mult, op1=ALU.add),
                             reads=[tb_], writes=[tb_])
                        S.op("pool", lambda e: e.tensor_tensor(out=t_[:, 0:n], in0=t_[:, 0:n], in1=x_[:, 0:n], op=ALU.mult), reads=[tb_, xb_], writes=[tb_])
                        s_, sb_ = g3.next()
                        S.op("act", lambda e: e.activation(out=s_[:, 0:n], in_=t_[:, 0:n], func=AF.Sigmoid, scale=2.0 * math.sqrt(2.0 / math.pi)), reads=[tb_], writes=[sb_])
                        S.op("dve", lambda e: e.tensor_tensor(out=out_ap, in0=x_[:, 0:n], in1=s_[:, 0:n], op=ALU.mult), reads=[xb_, sb_], writes=[outb])

                    for sl in range(2):
                        def evac_u(j, c0, n, ps, pb, sl=sl):
                            gelu(ps[:, 0:n], pb, n, UT[:, sl * 2 + j, c0:c0 + n], utb)
                        proj_fm(OFF_SGU + sl * 256, 256, evac_u)
                    wv = SB(ss_, "sgwv", [128, NCH, GW], BF16); wvb = Buf()
                    S.dma("pool", wv[:], w_in[l, :, OFF_SGV:OFF_SGV + GW].rearrange("(c p) n -> p c n", p=128), writes=[wvb])
                    wsT = SB(ss_, "sgwsT", [128, 4, 128], BF16); wsb = Buf()
                    S.dma("pool", wsT[:], sgwT_in[l], writes=[wsb])
                    lng = SB(ss_, "sglng", [128, GW]); lnb = SB(ss_, "sglnb", [128, GW]); bsr = SB(ss_, "sgbs", [128, 4, 128]); lnbuf = Buf()
                    S.dma("sp", lng[:], sglg_in[l], writes=[lnbuf])
                    S.dma("sp", lnb[:], sglb_in[l], writes=[lnbuf])
                    S.dma("sp", bsr[:], sgb_in[l], writes=[lnbuf])
                    vg = SB(ss_, "sgvg", [128, GW]); vgb = Buf()
                    st1 = SB(ss_, "sgst", [128, 4]); st1b = Buf()
                    vc = SB(ss_, "sgvc", [128, GW]); vcb = Buf()
                    vq = SB(ss_, "sgvq", [128, GW]); vqb = Buf()
                    vln = Rot(nc, ss_, "sgvln", [128, GW], BF16, 2)
                    tt_ = SB(ss_, "sgtt", [128, 4, 128]); ttb = Buf()
                    yo = Rot(nc, ss_, "sgyo", [128, 4, 128], BF16, 2)
                    for n_ in range(NTILE):
                        ps, pb = PS()
                        for k in range(NCH):
                            S.op("pe", lambda e: e.matmul(ps[:], lhsT=R[:, k, n_ * 128:(n_ + 1) * 128], rhs=wv[:, k, :], start=(k == 0), stop=(k == NCH - 1)),
                                 reads=[Rb, wvb], writes=[pb], inc=(k == NCH - 1))
                        gelu(ps[:], pb, GW, vg[:], vgb)
                        S.op("dve", lambda e: e.reduce_sum(out=st1[:, 0:1], in_=vg[:], axis=AX.X), reads=[vgb], writes=[st1b])
                        S.op("dve", lambda e: e.tensor_scalar(out=st1[:, 1:2], in0=st1[:, 0:1], scalar1=-1.0 / GW, scalar2=None, op0=ALU.mult), reads=[st1b], writes=[st1b])
                        S.op("dve", lambda e: e.tensor_scalar(out=vc[:], in0=vg[:], scalar1=st1[:, 1:2], scalar2=None, op0=ALU.add), reads=[vgb, st1b], writes=[vcb])
                        S.op("act", lambda e: e.activation(out=vq[:], in_=vc[:], func=AF.Square, accum_out=st1[:, 2:3]), reads=[vcb, st1b], writes=[vqb, st1b])
                        S.op("act", lambda e: e.activation(out=st1[:, 3:4], in_=st1[:, 2:3], func=AF.Sqrt, bias=EPS, scale=1.0 / GW), reads=[st1b], writes=[st1b])
                        S.op("dve", lambda e: e.reciprocal(out=st1[:, 3:4], in_=st1[:, 3:4]), reads=[st1b], writes=[st1b])
                        S.op("dve", lambda e: e.scalar_tensor_tensor(out=vc[:], in0=vc[:], scalar=st1[:, 3:4], in1=lng[:], op0=ALU.mult, op1=ALU.mult),
                             reads=[vcb, st1b, lnbuf], writes=[vcb])
                        vl, vlb = vln.next()
                        S.op("pool", lambda e: e.tensor_tensor(out=vl[:], in0=vc[:], in1=lnb[:], op=ALU.add), reads=[vcb, lnbuf], writes=[vlb])
                        ps2, pb2 = PS()
                        for h in range(4):
                            S.op("pe", lambda e: e.matmul(ps2[:, h * 128:(h + 1) * 128], lhsT=vl[:, h * 128:(h + 1) * 128], rhs=wsT[:, h, :], start=True, stop=True),
                                 reads=[vlb, wsb], writes=[pb2], inc=(h == 3))
                        S.op("dve", lambda e: e.tensor_tensor(out=tt_[:], in0=ps2[:].rearrange("p (h i) -> p h i", h=4), in1=bsr[:], op=ALU.add),
                             reads=[pb2, lnbuf], writes=[ttb])
                        yt, yb = yo.next()
                        S.op("pool", lambda e: e.tensor_tensor(out=yt[:], in0=tt_[:], in1=UT[:, :, n_ * 128:(n_ + 1) * 128], op=ALU.mult), reads=[ttb, utb], writes=[yb])
                        S.dma("sp", YT[512:1024, n_ * 128:(n_ + 1) * 128].rearrange("(h p) t -> p h t", p=128), yt[:], reads=[yb])
                    S.barrier()
                if stop_after == "B":
                    return
                with ExitStack() as sd_:
                    dn_scan(sd_, l, LA, BETA, NBETA, lab)
                    S.barrier()
                if stop_after == "C":
                    return
                with ExitStack() as sg2:
                    o0 = Rot(nc, sg2, "go0", [128, 4, 512], F32, 2)
                    o1 = Rot(nc, sg2, "go1", [128, 4, 512], F32, 2)
                    zr = Rot(nc, sg2, "gz", [128, 4, 512], BF16, 2)
                    oq = SB(sg2, "goq", [128, 4, 512]); oqb = Buf()
                    rr = Rot(nc, sg2, "grr", [128, 512], F32, 2)
                    yr = Rot(nc, sg2, "gy", [128, 4, 512], BF16, 2)
                    for (b0, bn) in BLOCKS:
                        a_, ab2 = o0.next(); b_, bb2 = o1.next(); z_, zb2 = zr.next()
                        S.dma("sp", a_[:, :, 0:bn], OSC[0, :, b0:b0 + bn].rearrange("(h p) t -> p h t", p=128), writes=[ab2])
                        S.dma("sp", b_[:, :, 0:bn], OSC[1, :, b0:b0 + bn].rearrange("(h p) t -> p h t", p=128), writes=[bb2])
                        S.dma("sp", z_[:, :, 0:bn], ZT[:, b0:b0 + bn].rearrange("(h p) t -> p h t", p=128), writes=[zb2])
                        S.op("dve", lambda e: e.tensor_tensor(out=a_[:, :, 0:bn], in0=a_[:, :, 0:bn], in1=b_[:, :, 0:bn], op=ALU.add), reads=[ab2, bb2], writes=[ab2])
                        S.op("act", lambda e: e.activation(out=oq[:, :, 0:bn], in_=a_[:, :, 0:bn], func=AF.Square), reads=[ab2], writes=[oqb])
                        yt, yb = yr.next()
                        for h in range(4):
                            ps, pb = PS()
                            S.op("pe", lambda e: e.matmul(ps[:, 0:bn], lhsT=ones32[:], rhs=oq[:, h, 0:bn], start=True, stop=True), reads=[oqb, onb], writes=[pb])
                            r_, rb_ = rr.next()
                            S.op("act", lambda e: e.activation(out=r_[:, 0:bn], in_=ps[:, 0:bn], func=AF.Sqrt, bias=EPS, scale=1.0 / 128), reads=[pb], writes=[rb_])
                            S.op("dve", lambda e: e.reciprocal(out=r_[:, 0:bn], in_=r_[:, 0:bn]), reads=[rb_], writes=[rb_])
                            S.op("dve", lambda e: e.tensor_tensor(out=r_[:, 0:bn], in0=r_[:, 0:bn], in1=a_[:, h, 0:bn], op=ALU.mult), reads=[rb_, ab2], writes=[rb_])
                            S.op("dve", lambda e: e.scalar_tensor_tensor(out=yt[:, h, 0:bn], in0=r_[:, 0:bn], scalar=small["dng"][0][:, l:l + 1], in1=z_[:, h, 0:bn],
                                                                          op0=ALU.mult, op1=ALU.mult), reads=[rb_, zb2, small["dng"][1]], writes=[yb])
                        S.dma("sp", YT[0:512, b0:b0 + bn].rearrange("(h p) t -> p h t", p=128), yt[:, :, 0:bn], reads=[yb])
                    S.barrier()
                if stop_after == "D":
                    return
                S.dma("sp", R[:], YT.rearrange("(c p) t -> p c t", p=128), writes=[Rb])
                with ExitStack() as se_:
                    wor = Rot(nc, se_, "wo", [128, NCH, 256], BF16, 3)
                    xr = Rot(nc, se_, "xo", [128, 512], F32, 3)
                    blocks = BLOCKS[:4] if last else BLOCKS
                    for sl in range(8):
                        wt, wb = wor.next()
                        S.dma("pool", wt[:], w_out[l, :, sl * 256:(sl + 1) * 256].rearrange("(c p) n -> p c n", p=128), writes=[wb])
                        for j in range(2):
                            c = sl * 2 + j
                            for (b0, bn) in blocks:
                                xt, xb = xr.next()
                                S.dma("sp", xt[:, 0:bn], XT[c * 128:(c + 1) * 128, b0:b0 + bn], writes=[xb])
                                ps, pb = PS()
                                for k in range(NCH):
                                    S.op("pe", lambda e: e.matmul(ps[:, 0:bn], lhsT=wt[:, k, j * 128:(j + 1) * 128], rhs=R[:, k, b0:b0 + bn],
                                                                  start=(k == 0), stop=(k == NCH - 1)), reads=[wb, Rb], writes=[pb], inc=(k == NCH - 1))
                                S.op("dve", lambda e: e.scalar_tensor_tensor(out=xt[:, 0:bn], in0=ps[:, 0:bn], scalar=mcol(l, 2, c, 0 if b0 < T else 1),
                                                                             in1=xt[:, 0:bn], op0=ALU.mult, op1=ALU.add), reads=[pb, xb, modb], writes=[xb])
                                S.dma("sp", XT[c * 128:(c + 1) * 128, b0:b0 + bn], xt[:, 0:bn], reads=[xb])
                    S.barrier()

        def dn_scan(sd_, l, LA, BETA, NBETA, lab):
            qk = Rot(nc, sd_, "dqkv", [128, 12, 128], BF16, 2)
            S32 = SB(sd_, "dS32", [128, 4, 128]); Sh = SB(sd_, "dSh", [128, 4, 128], BF16); s32b = Buf(); shb = Buf()
            T32 = lambda nm: (SB(sd_, nm, [128, 4, 128]), Buf())
            T16 = lambda nm: (SB(sd_, nm, [128, 4, 128], BF16), Buf())
            g8, g8b = SB(sd_, "dg8", [128, 8]), Buf()
            e8, e8b = SB(sd_, "de8", [128, 8]), Buf()
            sm, smb = SB(sd_, "dsm", [128, 12]), Buf()
            LT, LTb = T32("dLT"); Dm, Dmb = T32("dD"); eGj, eGjb = T16("deGj")
            Dn, Dnb = T32("dDn"); Dp, Dpb = T32("dDp"); dec, decb = T32("ddec"); decT, decTb = T32("ddecT")
            t1, t1b = T32("dt1")
            bV, bVb = T16("dbV"); KbE, KbEb = T16("dKbE"); KS, KSb = T16("dKS")
            qkmT, qkmTb = T16("dqkmT"); Qe, Qeb = T16("dQe"); nwT, nwTb = T16("dnwT")
            MTd, MTdb = T32("dMTd"); MTt, MTtb = T32("dMTt")
            _tc = {}

            def T32x(nm):
                if nm not in _tc:
                    _tc[nm] = T32(nm)
                return _tc[nm]
            AiT_t = SB(sd_, "dAiT", [128, 4, 128], BF16); AiT_b = Buf()
            vnb_, vnbb = T16("dvnb")
            ostg = Rot(nc, sd_, "dost", [128, 4, 128], F32, 2)
            v3 = lambda ap: ap.rearrange("p (h i) -> p h i", h=4)
            masks = SB(sd_, "masks", [128, 12, 4, 128]); mkb = Buf()
            S.dma("sp", masks[:], masks_in, writes=[mkb])
            mk = lambda i: masks[:, i, :, :]
            for dr in range(2):
                tri = mk(1) if dr == 0 else mk(2)
                lmask = mk(3) if dr == 0 else mk(4)
                qmask = mk(1) if dr == 0 else mk(2)
                tri1 = masks[:, 1 if dr == 0 else 2, 0, :]
                order = ([16, 17] + list(range(16))) if dr == 0 else ([17, 16] + list(range(15, -1, -1)))
                S.op("dve", lambda e: e.memset(S32[:], 0.0), writes=[s32b])
                S.op("dve", lambda e: e.memset(Sh[:], 0.0), writes=[shb])
                cs = slice(dr * 4, dr * 4 + 4)
                for n_ in order:
                    la = LA[:, n_, cs]; be = BETA[:, n_, cs]; nbe = NBETA[:, n_, cs]
                    bc = lambda ap: ap.unsqueeze(2).to_broadcast([128, 4, 128])
                    q_, qb_ = qk.next()
                    S.dma("sp", q_[:], QKVT[:, n_ * 128:(n_ + 1) * 128].rearrange("(c p) t -> p c t", p=128), writes=[qb_])
                    Qt = lambda h: q_[:, h, :]
                    Kt = lambda h: q_[:, 4 + h, :]
                    Vt = lambda h: q_[:, 8 + h, :]
                    ps, pb = PS()
                    S.op("pe", lambda e: e.matmul(ps[:, 0:4], lhsT=tri1, rhs=la, start=True, stop=True), reads=[mkb, lab], writes=[pb], inc=False)
                    S.op("pe", lambda e: e.matmul(ps[:, 4:8], lhsT=ones32[:], rhs=la, start=True, stop=True), reads=[onb, lab], writes=[pb])
                    S.op("dve", lambda e: e.tensor_copy(out=g8[:], in_=ps[:, 0:8]), reads=[pb], writes=[g8b])
                    S.op("act", lambda e: e.activation(out=e8[:], in_=ps[:, 0:8], func=AF.Exp), reads=[pb], writes=[e8b])
                    S.op("dve", lambda e: e.tensor_tensor(out=sm[:, 0:4], in0=e8[:, 0:4], in1=be, op=ALU.mult), reads=[e8b, lab], writes=[smb])
                    S.op("dve", lambda e: e.tensor_tensor(out=sm[:, 4:8], in0=g8[:, 4:8], in1=g8[:, 0:4], op=ALU.subtract), reads=[g8b], writes=[smb])
                    S.op("act", lambda e: e.activation(out=sm[:, 8:12], in_=sm[:, 4:8], func=AF.Exp), reads=[smb], writes=[smb])
                    S.op("dve", lambda e: e.tensor_tensor(out=LT[:], in0=tri, in1=bc(la), op=ALU.mult), reads=[mkb, lab], writes=[LTb])
                    psG, pbG = PS()
                    S.op("pe", lambda e: e.matmul(psG[:], lhsT=ones32[:], rhs=LT[:].rearrange("p h i -> p (h i)"), start=True, stop=True), reads=[onb, LTb], writes=[pbG])
                    S.op("dve", lambda e: e.tensor_tensor(out=Dm[:], in0=v3(psG[:]), in1=bc(g8[:, 0:4]), op=ALU.subtract), reads=[pbG, g8b], writes=[Dmb])
                    S.op("act", lambda e: e.activation(out=eGj[:], in_=v3(psG[:]), func=AF.Exp), reads=[pbG], writes=[eGjb])
                    S.op("pool", lambda e: e.tensor_scalar(out=Dn[:], in0=Dm[:], scalar1=0.0, scalar2=-1.0, op0=ALU.max, op1=ALU.mult), reads=[Dmb], writes=[Dnb])
                    S.op("pool", lambda e: e.tensor_scalar(out=Dp[:], in0=Dm[:], scalar1=0.0, scalar2=None, op0=ALU.min), reads=[Dmb], writes=[Dpb])
                    S.op("act", lambda e: e.activation(out=dec[:], in_=Dn[:], func=AF.Exp), reads=[Dnb], writes=[decb])
                    S.op("act", lambda e: e.activation(out=decT[:], in_=Dp[:], func=AF.Exp), reads=[Dpb], writes=[decTb])
                    S.op("pool", lambda e: e.tensor_tensor(out=dec[:], in0=dec[:], in1=lmask, op=ALU.mult), reads=[decb, mkb], writes=[decb])
                    S.op("pool", lambda e: e.tensor_tensor(out=decT[:], in0=decT[:], in1=qmask, op=ALU.mult), reads=[decTb, mkb], writes=[decTb])
                    pK, pKb = PSH()
                    for h in range(4):
                        S.op("pe", lambda e: e.transpose(pK[:, h * 128:(h + 1) * 128], Kt(h), identh), reads=[qb_, mkhb], writes=[pKb], inc=(h == 3))
                    S.op("dve", lambda e: e.tensor_tensor(out=KbE[:], in0=v3(pK[:, 0:512]), in1=bc(sm[:, 0:4]), op=ALU.mult), reads=[pKb, smb], writes=[KbEb])
                    S.op("dve", lambda e: e.tensor_tensor(out=KS[:], in0=v3(pK[:, 0:512]), in1=bc(sm[:, 8:12]), op=ALU.mult), reads=[pKb, smb], writes=[KSb])
                    pV, pVb = PSH()
                    for h in range(4):
                        S.op("pe", lambda e: e.transpose(pV[:, h * 128:(h + 1) * 128], Vt(h), identh), reads=[qb_, mkhb], writes=[pVb], inc=(h == 3))
                    S.op("dve", lambda e: e.tensor_tensor(out=bV[:], in0=v3(pV[:, 0:512]), in1=bc(be), op=ALU.mult), reads=[pVb, lab], writes=[bVb])
                    psK, pbK = PS()
                    for h in range(4):
                        S.op("pe", lambda e: e.matmul(psK[:, h * 128:(h + 1) * 128], lhsT=Kt(h), rhs=Kt(h), start=True, stop=True), reads=[qb_], writes=[pbK], inc=(h == 3))
                    psQ, pbQ = PS()
                    for h in range(4):
                        S.op("pe", lambda e: e.matmul(psQ[:, h * 128:(h + 1) * 128], lhsT=Kt(h), rhs=Qt(h), start=True, stop=True), reads=[qb_], writes=[pbQ], inc=(h == 3))
                    S.op("dve", lambda e: e.tensor_tensor(out=t1[:], in0=v3(psK[:]), in1=dec[:], op=ALU.mult), reads=[pbK, decb], writes=[t1b])
                    S.op("dve", lambda e: e.tensor_tensor(out=MTd[:], in0=t1[:], in1=bc(nbe), op=ALU.mult), reads=[t1b, lab], writes=[MTdb])
                    S.op("dve", lambda e: e.tensor_tensor(out=qkmT[:], in0=v3(psQ[:]), in1=decT[:], op=ALU.mult), reads=[pbQ, decTb], writes=[qkmTb])
                    S.op("pool", lambda e: e.tensor_tensor(out=Qe[:], in0=q_[:, 0:4, :], in1=eGj[:], op=ALU.mult), reads=[qb_, eGjb], writes=[Qeb])
                    psT, pbT = PS()
                    for h in range(4):
                        S.op("pe", lambda e: e.transpose(psT[:, h * 128:(h + 1) * 128], MTd[:, h, :], ident32), reads=[MTdb, idb], writes=[pbT], inc=(h == 3))
                    S.op("act", lambda e: e.copy(out=MTt[:], in_=v3(psT[:])), reads=[pbT], writes=[MTtb])
                    if dr == 0:
                        Nw, Nwb, NwT, NwTb = MTd, MTdb, MTt, MTtb
                    else:
                        NwT, NwTb, Nw, Nwb = MTd, MTdb, MTt, MTtb
                    G = {}
                    for s_, mi in ((16, 6), (32, 8), (64, 10)):
                        gt, gb = T32x("dG%d" % s_)
                        gtt, gtb = T32x("dGT%d" % s_)
                        if not (s_ == 64):
                            S.op("pool", lambda e: e.tensor_tensor(out=gt[:], in0=Nw[:], in1=mk(mi), op=ALU.mult), reads=[Nwb, mkb], writes=[gb])
                        S.op("pool", lambda e: e.tensor_tensor(out=gtt[:], in0=NwT[:], in1=mk(mi + 1), op=ALU.mult), reads=[NwTb, mkb], writes=[gtb])
                        G[s_] = (gt, gb, gtt, gtb)
                    Na, Nab = T32x("dNa"); NTa, NTab = T32x("dNTa")
                    S.op("dve", lambda e: e.tensor_tensor(out=Na[:], in0=Nw[:], in1=mk(5), op=ALU.mult), reads=[Nwb, mkb], writes=[Nab])
                    S.op("dve", lambda e: e.tensor_tensor(out=NTa[:], in0=NwT[:], in1=mk(5), op=ALU.mult), reads=[NwTb, mkb], writes=[NTab])
                    Di, Dib = T32x("dDi"); DiT, DiTb = T32x("dDiT")
                    S.op("pool", lambda e: e.tensor_tensor(out=Di[:], in0=Na[:], in1=mk(0), op=ALU.add), reads=[Nab, mkb], writes=[Dib])
                    S.op("pool", lambda e: e.tensor_tensor(out=DiT[:], in0=NTa[:], in1=mk(0), op=ALU.add), reads=[NTab, mkb], writes=[DiTb])
                    Fk, Fkb = T32x("dFk"); FkT, FkTb = T32x("dFkT")
                    Nb_, Nbb = T32x("dNb"); NTb_, NTbb = T32x("dNTb")
                    curN, curNb, curNT, curNTb = Na, Nab, NTa, NTab
                    othN, othNb, othNT, othNTb = Nb_, Nbb, NTb_, NTbb

                    def mm4(lhs, lhsb, rhs, rhsb):
                        p_, pb_ = PS()
                        for h in range(4):
                            S.op("pe", lambda e: e.matmul(p_[:, h * 128:(h + 1) * 128], lhsT=lhs[:, h, :], rhs=rhs[:, h, :], start=True, stop=True),
                                 reads=[lhsb, rhsb], writes=[pb_], inc=(h == 3))
                        return p_, pb_

                    for k in range(1, 4):
                        pA, pAb = mm4(curNT, curNTb, curN, curNb)
                        pB, pBb = mm4(curN, curNb, curNT, curNTb)
                        if k < 3:
                            S.op("act", lambda e: e.copy(out=othN[:], in_=v3(pA[:])), reads=[pAb], writes=[othNb])
                            S.op("act", lambda e: e.copy(out=othNT[:], in_=v3(pB[:])), reads=[pBb], writes=[othNTb])
                        S.op("dve", lambda e: e.tensor_tensor(out=Fk[:], in0=v3(pA[:]), in1=mk(0), op=ALU.add), reads=[pAb, mkb], writes=[Fkb])
                        S.op("dve", lambda e: e.tensor_tensor(out=FkT[:], in0=v3(pB[:]), in1=mk(0), op=ALU.add), reads=[pBb, mkb], writes=[FkTb])
                        pP, pPb = mm4(FkT, FkTb, Di, Dib)
                        pQ, pQb = mm4(Fk, Fkb, DiT, DiTb)
                        S.op("act", lambda e: e.copy(out=Di[:], in_=v3(pP[:])), reads=[pPb], writes=[Dib])
                        S.op("act", lambda e: e.copy(out=DiT[:], in_=v3(pQ[:])), reads=[pQb], writes=[DiTb])
                        curN, curNb, curNT, curNTb, othN, othNb, othNT, othNTb = othN, othNb, othNT, othNTb, curN, curNb, curNT, curNTb
                    T1, T1b = T32x("dT1")
                    for s_ in (16, 32, 64):
                        gt, gb, gtt, gtb = G[s_]
                        p1_, p1b_ = mm4(gtt, gtb, Di, Dib)
                        S.op("act", lambda e: e.copy(out=T1[:], in_=v3(p1_[:])), reads=[p1b_], writes=[T1b])
                        if s_ < 64 or dr == 1:
                            p3_, p3b_ = mm4(DiT, DiTb, T1, T1b)
                        if s_ < 64 or dr == 0:
                            p4_, p4b_ = mm4(T1, T1b, DiT, DiTb)
                        if s_ < 64 or dr == 1:
                            S.op("dve", lambda e: e.tensor_tensor(out=Di[:], in0=v3(p3_[:]), in1=Di[:], op=ALU.add), reads=[p3b_, Dib], writes=[Dib])
                        if s_ < 64 or dr == 0:
                            S.op("dve", lambda e: e.tensor_tensor(out=DiT[:], in0=v3(p4_[:]), in1=DiT[:], op=ALU.add), reads=[p4b_, DiTb], writes=[DiTb])
                    AiT, AiTb = AiT_t, AiT_b
                    fin, finb = (DiT, DiTb) if dr == 0 else (Di, Dib)
                    S.op("act", lambda e: e.copy(out=AiT[:], in_=fin[:]), reads=[finb], writes=[AiTb])
                    psW, pbW = PS()
                    for h in range(4):
                        S.op("pe", lambda e: e.matmul(psW[:, h * 128:(h + 1) * 128], lhsT=KbE[:, h, :], rhs=AiT[:, h, :], start=True, stop=True),
                             reads=[KbEb, AiTb], writes=[pbW], inc=(h == 3))
                    S.op("act", lambda e: e.activation(out=nwT[:], in_=v3(psW[:]), func=AF.Identity, scale=-1.0), reads=[pbW], writes=[nwTb])
                    psV, pbV = PS()
                    for h in range(4):
                        S.op("pe", lambda e: e.matmul(psV[:, h * 128:(h + 1) * 128], lhsT=AiT[:, h, :], rhs=bV[:, h, :], start=True, stop=False),
                             reads=[AiTb, bVb], writes=[pbV], inc=False)
                        S.op("pe", lambda e: e.matmul(psV[:, h * 128:(h + 1) * 128], lhsT=nwT[:, h, :], rhs=Sh[:, h, :], start=False, stop=True),
                             reads=[nwTb, shb], writes=[pbV], inc=(h == 3))
                    S.op("act", lambda e: e.copy(out=vnb_[:], in_=v3(psV[:])), reads=[pbV], writes=[vnbb])
                    psO, pbO = PS()
                    for h in range(4):
                        S.op("pe", lambda e: e.matmul(psO[:, h * 128:(h + 1) * 128], lhsT=Sh[:, h, :], rhs=Qe[:, h, :], start=True, stop=False),
                             reads=[shb, Qeb], writes=[pbO], inc=False)
                        S.op("pe", lambda e: e.matmul(psO[:, h * 128:(h + 1) * 128], lhsT=vnb_[:, h, :], rhs=qkmT[:, h, :], start=False, stop=True),
                             reads=[vnbb, qkmTb], writes=[pbO], inc=(h == 3))
                    ot, ob = ostg.next()
                    S.op("dve", lambda e: e.tensor_copy(out=ot[:], in_=v3(psO[:])), reads=[pbO], writes=[ob])
                    S.dma("sp", OSC[dr, :, n_ * 128:(n_ + 1) * 128].rearrange("(h p) t -> p h t", p=128), ot[:], reads=[ob])
                    psS, pbS = PS()
                    for h in range(4):
                        S.op("pe", lambda e: e.matmul(psS[:, h * 128:(h + 1) * 128], lhsT=KS[:, h, :], rhs=vnb_[:, h, :], start=True, stop=True),
                             reads=[KSb, vnbb], writes=[pbS], inc=(h == 3))
                    for h in range(4):
                        S.op("dve", lambda e: e.scalar_tensor_tensor(out=S32[:, h, :], in0=S32[:, h, :], scalar=e8[:, 4 + h:5 + h], in1=psS[:, h * 128:(h + 1) * 128],
                                                                     op0=ALU.mult, op1=ALU.add), reads=[s32b, e8b, pbS], writes=[s32b])
                    S.op("pool", lambda e: e.tensor_copy(out=Sh[:], in_=S32[:]), reads=[s32b], writes=[shb])

        def ffn_stage(l, moe, passes):
            with ExitStack() as st:
                NT = 1024
                X = SB(st, "fX", [128, NCH, NT]); Xb = [[Buf() for _ in range(2)] for _ in range(NCH)]
                Xall = [Xb[c][h] for c in range(NCH) for h in range(2)]
                H = SB(st, "fH", [128, NCH, NT], BF16); Hb = Buf()
                w13 = Rot(nc, st, "fw13", [128, NCH, 256], BF16, 3)
                w2r = Rot(nc, st, "fw2", [128, 2, D], BF16, 2)
                aT = Rot(nc, st, "faT", [128, 2, NT], BF16, 2)
                sr = Rot(nc, st, "fs", [128, 512], F32, 2)
                tr = Rot(nc, st, "ft", [128, 512], F32, 2)
                cB = Rot(nc, st, "fcB", [128, NT], F32, 1)
                for ps_ in passes:
                    kind, c0, n = ps_["kind"], ps_["c0"], ps_["n"]
                    w = 1 if kind == "ctx" else 0
                    halves = [(0, min(512, n))] + ([(512, n - 512)] if n > 512 else [])
                    if kind == "own":
                        with ExitStack() as s0:
                            xa = SB(s0, "fxa", [128, NT]); xab = Buf()
                            xb2 = SB(s0, "fxb", [128, NT]); xbb = Buf()
                            for c in range(NCH):
                                    S.dma("sp", xa[:], XT[c * 128:(c + 1) * 128, 0:NT], reads=[xtb], writes=[xab])
                                    S.dma("sp", xb2[:], XT[c * 128:(c + 1) * 128, NT:2 * NT], reads=[xtb], writes=[xbb])
                                    S.op("dve", lambda e: e.tensor_scalar(out=xa[:], in0=xa[:], scalar1=hmask[:, 0:1], scalar2=None, op0=ALU.mult), reads=[xab, hmb], writes=[xab])
                                    S.op("dve", lambda e: e.scalar_tensor_tensor(out=X[:, c, :], in0=xb2[:], scalar=hmask[:, 1:2], in1=xa[:], op0=ALU.mult, op1=ALU.add),
                                         reads=[xab, xbb, hmb], writes=Xb[c])
                            S.barrier()
                    with ExitStack() as s1:
                        if kind != "own":
                            S.dma("sp", X[:, :, 0:n], XT[:, c0:c0 + n].rearrange("(c p) t -> p c t", p=128), reads=[xtb], writes=Xall)
                        sqt = SB(s1, "fsq", [128, NCH, 128]); rst = SB(s1, "frs", [128, 512]); tmt = SB(s1, "ftm", [128, NCH, 128])
                        scr = (sqt, Buf(), rst, Buf(), tmt, Buf())
                        h32 = None
                        if moe:
                            h32 = (SB(s1, "fh32", [128, NCH, 128]), Buf())
                            CT = SB(s1, "fCT", [8, NT]); ctb = Buf()
                            rw = SB(s1, "frw", [128, NCH, NE]); rwb = Buf()
                            S.dma("sp", rw[:], router_w[0].rearrange("(c p) e -> p c e", p=128), writes=[rwb])
                            eex = SB(s1, "feex", [8, 128]); s8b = Buf()
                        for tt in range(n // 128):
                            t0_ = tt * 128
                            xbs = [Xb[c][t0_ // 512] for c in range(NCH)]
                            xsrc = X[:, :, t0_:t0_ + 128]
                            for b_ in xbs[1:]:
                                if b_.w is not None:
                                    S._wait("act", b_.w); S._wait("dve", b_.w)
                            if moe:
                                rms_block_h(l, 1, w, xsrc, xbs[0], 128, H, Hb, t0_, scr, h32)
                                psr, pbr = PS()
                                for k in range(NCH):
                                    S.op("pe", lambda e: e.matmul(psr[:, 0:NE], lhsT=h32[0][:, k, 0:128], rhs=rw[:, k, :],
                                                                  start=(k == 0), stop=(k == NCH - 1)), reads=[h32[1], rwb], writes=[pbr], inc=(k == NCH - 1))
                                route_tile(s1, psr, pbr, CT, ctb, t0_)
                            else:
                                rms_block(l, 1, w, xsrc, xbs[0], 128, H, Hb, t0_, scr)
                        ne = NE if moe else 1
                        dff = dffe if moe else DFF
                        for ex in range(ne):
                            W1 = (lambda a, b: moe_w1[0, ex, :, a:b]) if moe else (lambda a, b: ffn_w1[0, :, a:b])
                            W3 = (lambda a, b: moe_w3[0, ex, :, a:b]) if moe else (lambda a, b: ffn_w3[0, :, a:b])
                            W2 = (lambda a, b: moe_w2[0, ex, a:b, :]) if moe else (lambda a, b: ffn_w2[0, a:b, :])
                            if moe:
                                cb_, cbb = cB.next()
                                S.op("dve", lambda e: e.tensor_scalar(out=eex[:], in0=ones32[0:8, :], scalar1=ident32t[0:8, ex:ex + 1], scalar2=None, op0=ALU.mult),
                                     reads=[onb, idb], writes=[s8b])
                                for (h0, hn) in halves:
                                    psc, pbc = PS()
                                    S.op("pe", lambda e: e.matmul(psc[:, 0:hn], lhsT=eex[:], rhs=CT[:, h0:h0 + hn], start=True, stop=True),
                                         reads=[s8b, ctb], writes=[pbc])
                                    S.op("act", lambda e: e.copy(out=cb_[:, h0:h0 + hn], in_=psc[:, 0:hn]), reads=[pbc], writes=[cbb])
                            for sl in range(dff // 256):
                                w1t, w1b_ = w13.next()
                                S.dma("pool", w1t[:], W1(sl * 256, (sl + 1) * 256).rearrange("(c p) n -> p c n", p=128), writes=[w1b_])
                                w3t, w3b_ = w13.next()
                                S.dma("pool", w3t[:], W3(sl * 256, (sl + 1) * 256).rearrange("(c p) n -> p c n", p=128), writes=[w3b_])
                                w2t, w2b_ = w2r.next()
                                S.dma("pool", w2t[:], W2(sl * 256, (sl + 1) * 256).rearrange("(c p) n -> p c n", p=128), writes=[w2b_])
                                at, atb = aT.next()
                                for j in range(2):
                                    for (h0, hn) in halves:
                                        p1, p1b = PS()
                                        for k in range(NCH):
                                            S.op("pe", lambda e: e.matmul(p1[:, 0:hn], lhsT=w1t[:, k, j * 128:(j + 1) * 128], rhs=H[:, k, h0:h0 + hn],
                                                                          start=(k == 0), stop=(k == NCH - 1)), reads=[w1b_, Hb], writes=[p1b], inc=(k == NCH - 1))
                                        p3, p3b = PS()
                                        for k in range(NCH):
                                            S.op("pe", lambda e: e.matmul(p3[:, 0:hn], lhsT=w3t[:, k, j * 128:(j + 1) * 128], rhs=H[:, k, h0:h0 + hn],
                                                                          start=(k == 0), stop=(k == NCH - 1)), reads=[w3b_, Hb], writes=[p3b], inc=(k == NCH - 1))
                                        s_, sb_ = sr.next()
                                        S.op("act", lambda e: e.activation(out=s_[:, 0:hn], in_=p1[:, 0:hn], func=AF.Silu), reads=[p1b], writes=[sb_])
                                        if moe:
                                            t_, tb_ = tr.next()
                                            S.op("dve", lambda e: e.tensor_tensor(out=t_[:, 0:hn], in0=p3[:, 0:hn], in1=cb_[:, h0:h0 + hn], op=ALU.mult), reads=[p3b, cbb], writes=[tb_])
                                            S.op("pool", lambda e: e.tensor_tensor(out=at[:, j, h0:h0 + hn], in0=s_[:, 0:hn], in1=t_[:, 0:hn], op=ALU.mult), reads=[sb_, tb_], writes=[atb])
                                        else:
                                            S.op("dve", lambda e: e.tensor_tensor(out=at[:, j, h0:h0 + hn], in0=p3[:, 0:hn], in1=s_[:, 0:hn], op=ALU.mult), reads=[p3b, sb_], writes=[atb])
                                for c in range(NCH):
                                    for (h0, hn) in halves:
                                        p2, p2b = PS()
                                        for j in range(2):
                                            S.op("pe", lambda e: e.matmul(p2[:, 0:hn], lhsT=w2t[:, j, c * 128:(c + 1) * 128], rhs=at[:, j, h0:h0 + hn],
                                                                          start=(j == 0), stop=(j == 1)), reads=[w2b_, atb], writes=[p2b], inc=(j == 1))
                                        xb_ = Xb[c][h0 // 512]
                                        S.op("dve", lambda e: e.scalar_tensor_tensor(out=X[:, c, h0:h0 + hn], in0=p2[:, 0:hn], scalar=mcol(l, 5, c, w), in1=X[:, c, h0:h0 + hn],
                                                                                     op0=ALU.mult, op1=ALU.add), reads=[p2b, xb_, modb], writes=[xb_])
                        if kind == "own":
                            fo = Rot(nc, s1, "fo", [128, 512], F32, 2)
                            for (h0, hn) in halves:
                                xbs = [Xb[c][h0 // 512] for c in range(NCH)]
                                for b_ in xbs:
                                    S._wait("act", b_.w); S._wait("dve", b_.w)
                                psn, pbn = PS()
                                for tt in range(hn // 128):
                                    S.op("act", lambda e: e.activation(out=sqt[:, :, 0:128], in_=X[:, :, h0 + tt * 128:h0 + (tt + 1) * 128], func=AF.Square), reads=[xbs[0]], writes=[scr[1]])
                                    for k in range(NCH):
                                        S.op("pe", lambda e: e.matmul(psn[:, tt * 128:(tt + 1) * 128], lhsT=ones32[:], rhs=sqt[:, k, 0:128], start=(k == 0), stop=(k == NCH - 1)),
                                             reads=[scr[1], onb], writes=[pbn], inc=(k == NCH - 1))
                                S.op("act", lambda e: e.activation(out=rst[:, 0:hn], in_=psn[:, 0:hn], func=AF.Sqrt, bias=EPS, scale=1.0 / D), reads=[pbn], writes=[scr[3]])
                                S.op("dve", lambda e: e.reciprocal(out=rst[:, 0:hn], in_=rst[:, 0:hn]), reads=[scr[3]], writes=[scr[3]])
                                for c in range(NCH):
                                    ot, ob = fo.next()
                                    S.op("dve", lambda e: e.scalar_tensor_tensor(out=ot[:, 0:hn], in0=X[:, c, h0:h0 + hn], scalar=small["fng"][0][:, c:c + 1], in1=rst[:, 0:hn],
                                                                                 op0=ALU.mult, op1=ALU.mult), reads=[xbs[c], scr[3], small["fng"][1]], writes=[ob])
                                    S.out_toks.append(S.dma("sp", outT[c * 128:(c + 1) * 128, h0:h0 + hn], ot[:, 0:hn], reads=[ob]))
                        else:
                            S.dma("sp", XT[:, c0:c0 + n].rearrange("(c p) t -> p c t", p=128), X[:, :, 0:n], reads=Xall, writes=[xtb])
                        S.barrier()

        def rms_block_h(l, fi, w, xt, xb, n, R, Rb, dcol, scr, h32):
            sqt, sqb, rst, rsb, tmt, tmb = scr
            S.op("act", lambda e: e.activation(out=sqt[:, :, 0:n], in_=xt, func=AF.Square), reads=[xb], writes=[sqb])
            ps, pb = PS()
            for k in range(NCH):
                S.op("pe", lambda e: e.matmul(ps[:, 0:n], lhsT=ones32[:], rhs=sqt[:, k, 0:n], start=(k == 0), stop=(k == NCH - 1)),
                     reads=[sqb, onb], writes=[pb], inc=(k == NCH - 1))
            S.op("act", lambda e: e.activation(out=rst[:, 0:n], in_=ps[:, 0:n], func=AF.Sqrt, bias=EPS, scale=1.0 / D), reads=[pb], writes=[rsb])
            S.op("dve", lambda e: e.reciprocal(out=rst[:, 0:n], in_=rst[:, 0:n]), reads=[rsb], writes=[rsb])
            S.op("dve", lambda e: e.tensor_tensor(out=tmt[:, :, 0:n], in0=xt, in1=rst[:, 0:n].unsqueeze(1).to_broadcast([128, NCH, n]), op=ALU.mult),
                 reads=[xb, rsb], writes=[tmb])
            for k in range(NCH):
                S.op("act", lambda e: e.activation(out=h32[0][:, k, 0:n], in_=tmt[:, k, 0:n], func=AF.Identity,
                                                   bias=mcol(l, 3, k, w), scale=GS[:, l, fi, k, w:w + 1]), reads=[tmb, modb, gsb], writes=[h32[1]])
            S.op("dve", lambda e: e.tensor_copy(out=R[:, :, dcol:dcol + n], in_=h32[0][:, :, 0:n]), reads=[h32[1]], writes=[Rb])

        rt_state = {}

        def route_tile(s1, psr, pbr, CT, ctb, tcol):
            if "lg" not in rt_state or rt_state["s1"] is not s1:
                rt_state["s1"] = s1
                rt_state["lg"] = (SB(s1, "rlg", [128, NE]), Buf())
                rt_state["mx"] = (SB(s1, "rmx", [128, 8]), Buf())
                rt_state["sel"] = (SB(s1, "rsel", [128, NE]), Buf())
                rt_state["ex"] = (SB(s1, "rex", [128, NE]), Buf())
                rt_state["r"] = (SB(s1, "rr_", [128, 2]), Buf())
                rt_state["cm"] = (SB(s1, "rcm", [128, NE]), Buf())
            lg, lgb = rt_state["lg"]; mx, mxb = rt_state["mx"]; sel, selb = rt_state["sel"]
            ex, exb = rt_state["ex"]; r_, rb_ = rt_state["r"]; cm, cmb = rt_state["cm"]
            S.op("dve", lambda e: e.tensor_tensor(out=lg[:], in0=psr[:, 0:NE], in1=small["rb"][0][:], op=ALU.add), reads=[pbr, small["rb"][1]], writes=[lgb])
            S.op("dve", lambda e: e.max(out=mx[:], in_=lg[:]), reads=[lgb], writes=[mxb])
            S.op("dve", lambda e: e.tensor_scalar(out=sel[:], in0=lg[:], scalar1=mx[:, 1:2], scalar2=None, op0=ALU.is_ge), reads=[lgb, mxb], writes=[selb])
            S.op("dve", lambda e: e.tensor_scalar(out=ex[:], in0=lg[:], scalar1=mx[:, 0:1], scalar2=None, op0=ALU.subtract), reads=[lgb, mxb], writes=[exb])
            S.op("act", lambda e: e.activation(out=ex[:], in_=ex[:], func=AF.Exp), reads=[exb], writes=[exb])
            S.op("dve", lambda e: e.tensor_tensor(out=r_[:, 0:1], in0=mx[:, 1:2], in1=mx[:, 0:1], op=ALU.subtract), reads=[mxb], writes=[rb_])
            S.op("act", lambda e: e.activation(out=r_[:, 0:1], in_=r_[:, 0:1], func=AF.Exp), reads=[rb_], writes=[rb_])
            S.op("dve", lambda e: e.tensor_scalar(out=r_[:, 0:1], in0=r_[:, 0:1], scalar1=1.0, scalar2=None, op0=ALU.add), reads=[rb_], writes=[rb_])
            S.op("dve", lambda e: e.reciprocal(out=r_[:, 1:2], in_=r_[:, 0:1]), reads=[rb_], writes=[rb_])
            S.op("dve", lambda e: e.scalar_tensor_tensor(out=cm[:], in0=ex[:], scalar=r_[:, 1:2], in1=sel[:], op0=ALU.mult, op1=ALU.mult), reads=[exb, rb_, selb], writes=[cmb])
            pst, pstb = PS()
            S.op("pe", lambda e: e.transpose(pst[0:NE, 0:128], cm[:], ident32), reads=[cmb, idb], writes=[pstb])
            S.op("act", lambda e: e.copy(out=CT[:, tcol:tcol + 128], in_=pst[0:NE, 0:128]), reads=[pstb], writes=[ctb])

        if stop_after == "MOE":
            ffn_stage(1, True, [dict(kind="own", c0=0, n=1024)])
        else:
            mixing_layer(0)
        if stop_after is None or stop_after == "F0":
            ffn_stage(0, False, [dict(kind="x", c0=0, n=1024), dict(kind="x", c0=1024, n=1024), dict(kind="ctx", c0=T, n=TC)])
        if stop_after is None and layers == 2:
            mixing_layer(1)
            ffn_stage(1, True, [dict(kind="own", c0=0, n=1024)])
        S.barrier()
        for t in S.out_toks:
            S._wait("sp", t)
        build_nc.nops = S.nops
    return nc


def _pos_embed_T():
    rows = T // 64
    r = np.repeat(np.arange(rows, dtype=np.float32), 64)
    col = np.tile(np.arange(64, dtype=np.float32), rows)
    quarter = D // 4
    freq = np.exp(np.float32(-math.log(10000.0)) * np.arange(quarter, dtype=np.float32) / np.float32(quarter)).astype(np.float32)
    ar = r[:, None] * freq
    ac = col[:, None] * freq
    pe = np.concatenate([np.sin(ar), np.cos(ar), np.sin(ac), np.cos(ac)], axis=-1).astype(np.float32)
    return np.ascontiguousarray(pe.T)


def _consts():
    p = np.arange(128)[:, None]
    f = np.arange(128)[None, :]
    ml = [(p == f), (p <= f), (p >= f), (p > f), (p < f), (p // 16 == f // 16)]
    for s_ in (16, 32, 64):
        ms = (p // (2 * s_) == f // (2 * s_)) & (p % (2 * s_) >= s_) & (f % (2 * s_) < s_)
        ml += [ms, ms.T]
    m = np.stack(ml).astype(np.float32)
    masks = np.ascontiguousarray(np.broadcast_to(m.transpose(1, 0, 2)[:, :, None, :], (128, 12, 4, 128))).astype(np.float32)
    inv = np.zeros((4, PW), np.float32)
    for gi, w in enumerate((2, 4, 8, 16)):
        for (off, L) in ((XOFF, T), (COFF, TC)):
            t = np.arange(L)
            lo = np.clip(t - w // 2, 0, L)
            hi = np.clip(t + w // 2, 0, L)
            inv[gi, off:off + L] = 1.0 / (hi - lo).astype(np.float32)
    invcnt = np.ascontiguousarray(np.broadcast_to(inv[:, None, :], (4, 128, PW))).astype(np.float32)
    sel8 = np.zeros((8, NE, 128), np.float32)
    for e in range(NE):
        sel8[e, e, :] = 1.0
    return masks, invcnt, sel8


def _pc(v, n):
    return np.ascontiguousarray(np.asarray(v, np.float32).reshape(n, 128).T)


def prep_inputs(inputs, r):
    g = {k: np.asarray(v) for k, v in inputs.items()}
    b, half = r // 2, r % 2
    f32 = np.float32
    masks, invcnt, sel8 = _consts()
    m = {}
    m["xT"] = np.ascontiguousarray(g["x"][b].T).astype(f32)
    m["ctxT"] = np.ascontiguousarray(g["ctx"][b].T).astype(f32)
    m["posT"] = _pos_embed_T()
    m["c2"] = np.ascontiguousarray(np.stack([_pc(g["c"][b], NCH), _pc(g["c_ctx"], NCH)], axis=-1))
    hm = np.zeros((128, 2), f32); hm[:, half] = 1.0
    m["hmask"] = hm
    m["ada_w"] = g["ada_w"]
    m["ada_bT"] = np.ascontiguousarray(np.stack([_pc(g["ada_b"][l], 96) for l in range(2)], axis=1))
    m["norm_mix_gT"] = np.ascontiguousarray(np.stack([_pc(g["norm_mix_g"][l], NCH) for l in range(2)], axis=1))
    m["norm_ffn_gT"] = np.ascontiguousarray(np.stack([_pc(g["norm_ffn_g"][l], NCH) for l in range(2)], axis=1))
    m["final_norm_gT"] = _pc(g["final_norm_g"], NCH)
    m["w_in"] = g["w_in"]; m["w_out"] = g["w_out"]
    m["dn_conv_wT"] = np.ascontiguousarray(g["dn_conv_w"].reshape(2, 5, 12, 128).transpose(3, 0, 2, 1)).astype(f32)
    m["dn_a_log_r"] = np.ascontiguousarray(np.broadcast_to(g["dn_a_log"].reshape(1, 2, 8), (128, 2, 8))).astype(f32)
    m["dn_dt_bias_r"] = np.ascontiguousarray(np.broadcast_to(g["dn_dt_bias"].reshape(1, 2, 8), (128, 2, 8))).astype(f32)
    m["dn_norm_gT"] = np.ascontiguousarray(g["dn_norm_g"].T).astype(f32)
    m["sg_ln_g_r"] = np.ascontiguousarray(np.broadcast_to(g["sg_ln_g"][:, None, :], (2, 128, GW))).astype(f32)
    m["sg_ln_b_r"] = np.ascontiguousarray(np.broadcast_to(g["sg_ln_b"][:, None, :], (2, 128, GW))).astype(f32)
    m["sg_wT"] = np.ascontiguousarray(g["sg_w"].transpose(0, 3, 1, 2)).astype(f32)
    m["sg_b_r"] = np.ascontiguousarray(np.broadcast_to(g["sg_b"][:, None, :, :], (2, 128, 4, 128))).astype(f32)
    m["pool_wT"] = np.ascontiguousarray(g["pool_w"].transpose(0, 2, 1, 3)).astype(f32)
    m["pool_scaleT"] = np.ascontiguousarray(np.stack([_pc(g["pool_scale"][l], 4) for l in range(2)], axis=1))
    m["cv_wT"] = np.ascontiguousarray(g["cv_w"].reshape(2, 31, 4, 128).transpose(3, 0, 2, 1)).astype(f32)
    for nm in ("cv_b", "cv_ln_g", "cv_ln_b"):
        m[nm + "T"] = np.ascontiguousarray(np.stack([_pc(g[nm][l], 4) for l in range(2)], axis=1))
    for nm in ("ffn_w1", "ffn_w3", "ffn_w2", "router_w", "moe_w1", "moe_w3", "moe_w2"):
        m[nm] = g[nm]
    m["router_b_r"] = np.ascontiguousarray(np.broadcast_to(g["router_b"].reshape(1, NE), (128, NE))).astype(f32)
    m["masks"] = masks; m["invcnt"] = invcnt; m["sel8"] = sel8
    return m


def kernel(**inputs):
    nc = build_nc()
    in_maps = [prep_inputs(inputs, r) for r in range(8)]
    res = run_bass_kernel_spmd(nc, in_maps, core_ids=list(range(8)))
    out = np.zeros((4, T, D), np.float32)
    for r in range(8):
        b, half = r // 2, r % 2
        out[b, half * 1024:(half + 1) * 1024, :] = res.results[r]["outT"].T
    return out
```
